# Optimizing a Trainium2 kernel written in Bass

```python
import math
import jax, jax.numpy as jnp
from jax import lax
import numpy as np

D_MODEL = 1024
BATCH = 2
SEQ = 8192
DEPTH = 1

CHUNK = 64
Q_BLOCK = 128
FOX_HEADS = 8
FOX_HEAD_DIM = 64
FOX_WIDTH = FOX_HEADS * FOX_HEAD_DIM
DIFF_HEADS = 4
DIFF_HEAD_DIM = 64
DIFF_V_DIM = 2 * DIFF_HEAD_DIM
DIFF_WIDTH = DIFF_HEADS * DIFF_V_DIM
N_BRANCH = 2
IN_COLS = 3 * FOX_WIDTH + FOX_HEADS + 3 * DIFF_WIDTH + N_BRANCH * D_MODEL
N_GROUPS = 4
EXPERTS_PER_GROUP = 8
N_EXPERTS = N_GROUPS * EXPERTS_PER_GROUP
TOP_K_EXPERT = 2
D_EXPERT = D_MODEL // 2
MOE_BLOCK = 128
EPS = 1e-6

kernel_name = "fox_diffattn_hiermoe_hybrid_block"


def rms_norm(x, g):
    xf = x.astype(jnp.float32)
    y = xf * lax.rsqrt(jnp.mean(xf * xf, axis=-1, keepdims=True) + EPS)
    return (y * g.astype(jnp.float32)).astype(x.dtype)


def to_heads(t, n_heads):
    b, s, _ = t.shape
    return t.reshape(b, s, n_heads, -1).transpose(0, 2, 1, 3)


def from_heads(t):
    b, h, s, d = t.shape
    return t.transpose(0, 2, 1, 3).reshape(b, s, h * d)


def fox_attention(q, k, v, log_f):
    B, H, S, d = q.shape
    F = jnp.cumsum(log_f, axis=-1)
    pos = jnp.arange(S)
    scale = d ** -0.5

    def block(i):
        start = i * Q_BLOCK
        qb = lax.dynamic_slice_in_dim(q, start, Q_BLOCK, axis=2)
        Fq = lax.dynamic_slice_in_dim(F, start, Q_BLOCK, axis=2)
        tq = start + jnp.arange(Q_BLOCK)
        s = jnp.einsum('bhqd,bhkd->bhqk', qb, k, preferred_element_type=jnp.float32) * scale
        s = s + Fq[..., :, None] - F[..., None, :]
        mask = pos[None, :] <= tq[:, None]
        s = jnp.where(mask, s, -jnp.inf)
        p = jax.nn.softmax(s, axis=-1).astype(v.dtype)
        return jnp.einsum('bhqk,bhkd->bhqd', p, v)

    out = lax.map(block, jnp.arange(S // Q_BLOCK))
    return out.transpose(1, 2, 0, 3, 4).reshape(B, H, S, d)


def diff_attention(q1, q2, k1, k2, v, lam, slopes):
    B, H, S, d = q1.shape
    pos = jnp.arange(S)
    chunk_k = pos // CHUNK
    scale = d ** -0.5

    def block(i):
        start = i * Q_BLOCK
        q1b = lax.dynamic_slice_in_dim(q1, start, Q_BLOCK, axis=2)
        q2b = lax.dynamic_slice_in_dim(q2, start, Q_BLOCK, axis=2)
        tq = start + jnp.arange(Q_BLOCK)
        dist = jnp.abs(tq[:, None] - pos[None, :]).astype(jnp.float32)
        bias = -slopes[:, None, None] * dist
        mask = chunk_k[None, :] <= (tq // CHUNK)[:, None]
        s1 = jnp.einsum('bhqd,bhkd->bhqk', q1b, k1, preferred_element_type=jnp.float32) * scale + bias
        s2 = jnp.einsum('bhqd,bhkd->bhqk', q2b, k2, preferred_element_type=jnp.float32) * scale + bias
        p1 = jax.nn.softmax(jnp.where(mask, s1, -jnp.inf), axis=-1)
        p2 = jax.nn.softmax(jnp.where(mask, s2, -jnp.inf), axis=-1)
        a = (p1 - lam * p2).astype(v.dtype)
        return jnp.einsum('bhqk,bhkd->bhqd', a, v)

    out = lax.map(block, jnp.arange(S // Q_BLOCK))
    return out.transpose(1, 2, 0, 3, 4).reshape(B, H, S, v.shape[-1])


def hybrid_mixer(h, w_in, b_fgate, b_gate, lq1, lk1, lq2, lk2, g_subln, w_pa, w_pb, w_o, lam_init):
    z = h @ w_in
    widths = [FOX_WIDTH] * 3 + [FOX_HEADS] + [DIFF_WIDTH] * 3 + [D_MODEL] * N_BRANCH
    splits = [int(c) for c in np.cumsum(widths)[:-1]]
    fq, fk, fv, ff, dq, dk, dv, ga, gb = jnp.split(z, splits, axis=-1)

    log_f = jax.nn.log_sigmoid((ff + b_fgate).astype(jnp.float32)).transpose(0, 2, 1)
    o_a = from_heads(fox_attention(to_heads(fq, FOX_HEADS), to_heads(fk, FOX_HEADS),
                                   to_heads(fv, FOX_HEADS), log_f))

    qh = to_heads(dq, DIFF_HEADS)
    kh = to_heads(dk, DIFF_HEADS)
    vh = to_heads(dv, DIFF_HEADS)
    q1, q2 = qh[..., :DIFF_HEAD_DIM], qh[..., DIFF_HEAD_DIM:]
    k1, k2 = kh[..., :DIFF_HEAD_DIM], kh[..., DIFF_HEAD_DIM:]
    f32 = jnp.float32
    lam = (jnp.exp(jnp.sum(lq1.astype(f32) * lk1.astype(f32)))
           - jnp.exp(jnp.sum(lq2.astype(f32) * lk2.astype(f32))) + lam_init)
    slopes = jnp.exp2(-8.0 / DIFF_HEADS * jnp.arange(1, DIFF_HEADS + 1, dtype=f32))
    ob = diff_attention(q1, q2, k1, k2, vh, lam, slopes)
    ob = rms_norm(ob, g_subln) * (1.0 - lam_init)
    o_b = from_heads(ob)

    gate_a = jax.nn.sigmoid(ga + b_gate[:D_MODEL])
    gate_b = jax.nn.sigmoid(gb + b_gate[D_MODEL:])
    y = gate_a * (o_a @ w_pa) + gate_b * (o_b @ w_pb)
    return y @ w_o


def hier_moe(h, w_group, b_group, w_expert, b_expert, w1, w3, w2):
    B, S, D = h.shape
    N = B * S
    hf = h.reshape(N, D)
    f32 = jnp.float32
    gl = (hf @ w_group).astype(f32) + b_group.astype(f32)
    gp = jax.nn.softmax(gl, axis=-1)
    g_idx = jnp.argmax(gl, axis=-1)
    g_w = jnp.take_along_axis(gp, g_idx[:, None], axis=-1)[:, 0]
    el = (hf @ w_expert).astype(f32).reshape(N, N_GROUPS, EXPERTS_PER_GROUP)
    el = jnp.take_along_axis(el, g_idx[:, None, None], axis=1)[:, 0] + b_expert.astype(f32)[g_idx]
    ep = jax.nn.softmax(el, axis=-1)
    vals, idx = lax.top_k(ep, TOP_K_EXPERT)
    vals = vals / jnp.sum(vals, axis=-1, keepdims=True)
    weights = g_w[:, None] * vals
    expert_ids = (g_idx[:, None] * EXPERTS_PER_GROUP + idx).astype(jnp.int32)

    A = N * TOP_K_EXPERT
    e_flat = expert_ids.reshape(A)
    w_flat = weights.reshape(A)
    tok = jnp.repeat(jnp.arange(N, dtype=jnp.int32), TOP_K_EXPERT)
    order = jnp.argsort(e_flat)
    e_s, tok_s, w_s = e_flat[order], tok[order], w_flat[order]
    counts = jnp.bincount(e_flat, length=N_EXPERTS)
    padded = (counts + MOE_BLOCK - 1) // MOE_BLOCK * MOE_BLOCK
    start = jnp.cumsum(counts) - counts
    pstart = jnp.cumsum(padded) - padded
    dest = pstart[e_s] + jnp.arange(A) - start[e_s]
    P = A + N_EXPERTS * MOE_BLOCK
    n_blk = P // MOE_BLOCK
    row_tok = jnp.zeros((P,), jnp.int32).at[dest].set(tok_s)
    row_w = jnp.zeros((P,), f32).at[dest].set(w_s)
    blk_e = jnp.searchsorted(jnp.cumsum(padded), jnp.arange(n_blk) * MOE_BLOCK, side='right')
    blk_e = jnp.minimum(blk_e, N_EXPERTS - 1)
    xb = hf[row_tok].reshape(n_blk, MOE_BLOCK, D)

    def expert_block(args):
        xblk, e = args
        return (jax.nn.silu(xblk @ w1[e]) * (xblk @ w3[e])) @ w2[e]

    yb = lax.map(expert_block, (xb, blk_e)).reshape(P, D)
    out = jnp.zeros_like(hf).at[row_tok].add(yb * row_w[:, None].astype(yb.dtype))
    return out.reshape(B, S, D)


def setup_inputs(seed: int = 0) -> dict:
    key = jax.random.key(seed)
    ks = jax.random.split(key, 24)
    n = jax.random.normal
    f = jnp.float32
    L = DEPTH
    return {
        "x": n(ks[0], (BATCH, SEQ, D_MODEL), f),
        "g_mix": 1.0 + 0.01 * n(ks[1], (L, D_MODEL), f),
        "w_in": n(ks[2], (L, D_MODEL, IN_COLS), f) * D_MODEL ** -0.5,
        "b_fgate": 1.0 + 0.5 * n(ks[3], (L, FOX_HEADS), f),
        "b_gate": 0.1 * n(ks[4], (L, N_BRANCH * D_MODEL), f),
        "lam_q1": 0.1 * n(ks[5], (L, DIFF_HEAD_DIM), f),
        "lam_k1": 0.1 * n(ks[6], (L, DIFF_HEAD_DIM), f),
        "lam_q2": 0.1 * n(ks[7], (L, DIFF_HEAD_DIM), f),
        "lam_k2": 0.1 * n(ks[8], (L, DIFF_HEAD_DIM), f),
        "g_subln": 1.0 + 0.01 * n(ks[9], (L, DIFF_V_DIM), f),
        "w_pa": n(ks[10], (L, FOX_WIDTH, D_MODEL), f) * FOX_WIDTH ** -0.5,
        "w_pb": n(ks[11], (L, DIFF_WIDTH, D_MODEL), f) * DIFF_WIDTH ** -0.5,
        "w_o": n(ks[12], (L, D_MODEL, D_MODEL), f) * D_MODEL ** -0.5,
        "g_moe": 1.0 + 0.01 * n(ks[13], (L, D_MODEL), f),
        "w_group": n(ks[14], (L, D_MODEL, N_GROUPS), f) * D_MODEL ** -0.5,
        "b_group": 0.01 * n(ks[15], (L, N_GROUPS), f),
        "w_expert": n(ks[16], (L, D_MODEL, N_EXPERTS), f) * D_MODEL ** -0.5,
        "b_expert": 0.01 * n(ks[17], (L, N_GROUPS, EXPERTS_PER_GROUP), f),
        "w1": n(ks[18], (L, N_EXPERTS, D_MODEL, D_EXPERT), f) * D_MODEL ** -0.5,
        "w3": n(ks[19], (L, N_EXPERTS, D_MODEL, D_EXPERT), f) * D_MODEL ** -0.5,
        "w2": n(ks[20], (L, N_EXPERTS, D_EXPERT, D_MODEL), f) * D_EXPERT ** -0.5,
        "g_final": 1.0 + 0.01 * n(ks[21], (D_MODEL,), f),
    }


def reference(x, g_mix, w_in, b_fgate, b_gate, lam_q1, lam_k1, lam_q2, lam_k2, g_subln,
              w_pa, w_pb, w_o, g_moe, w_group, b_group, w_expert, b_expert, w1, w3, w2, g_final):
    for l in range(DEPTH):
        lam_init = 0.8 - 0.6 * math.exp(-0.3 * l)
        h = rms_norm(x, g_mix[l])
        x = x + hybrid_mixer(h, w_in[l], b_fgate[l], b_gate[l], lam_q1[l], lam_k1[l],
                             lam_q2[l], lam_k2[l], g_subln[l], w_pa[l], w_pb[l], w_o[l], lam_init)
        h = rms_norm(x, g_moe[l])
        x = x + hier_moe(h, w_group[l], b_group[l], w_expert[l], b_expert[l], w1[l], w3[l], w2[l])
    return rms_norm(x, g_final)
```

```python
import contextlib
import numpy as np
import concourse.bass as bass
import concourse.mybir as mybir
from concourse.bass_utils import run_bass_kernel_spmd

F32 = mybir.dt.float32
I32 = mybir.dt.int32
BF16 = mybir.dt.bfloat16
AF = mybir.ActivationFunctionType
ALU = mybir.AluOpType
AX = mybir.AxisListType

D = 1024
SEQ = 8192
NQ = 2048
EPS = 1e-6
NEG = -1.0e30
LAM_INIT = 0.2
C_FQ, C_FK, C_FV, C_FF, C_DQ, C_DK, C_DV, C_GA, C_GB = 0, 512, 1024, 1536, 1544, 2056, 2568, 3080, 4104
NDMA = 8
NBLK = 48
NROWS = NBLK * 256


class Op:
    __slots__ = ("eng", "emit", "deps", "sig", "tok", "is_dma", "pre")

    def __init__(self, eng, emit, is_dma=False):
        self.eng = eng
        self.emit = emit
        self.deps = []
        self.sig = False
        self.tok = None
        self.is_dma = is_dma
        self.pre = None


class Sched:
    ENG = ("pe", "act", "dve", "pool", "sp")

    def __init__(self, nc, stack):
        self.nc = nc
        self.sem = {e: stack.enter_context(nc.semaphore("s_" + e)) for e in ("pe", "act", "dve", "pool")}
        self.cnt = {e: 0 for e in self.sem}
        self.dsem = {q: [stack.enter_context(nc.semaphore("d_%s%d" % (q, i))) for i in range(NDMA)]
                     for q in ("sp", "pool")}
        self.dcnt = {q: [0] * NDMA for q in ("sp", "pool")}
        self.dnext = {q: 0 for q in ("sp", "pool")}
        self.waited = {e: {} for e in self.ENG}
        self.regcache = {}
        self.ops_done = []
        self.reset()

    def reset(self):
        self.ops = []
        self.last_w = {}
        self.readers = {}
        self.multi_w = {}

    def _add(self, op, reads, writes):
        deps = []
        for k in reads:
            if k in ("W", "Wk", "Wv", "Wf"):
                deps.extend(self.multi_w.get(k, ()))
                continue
            w = self.last_w.get(k)
            if w is not None:
                deps.append(w)
            self.readers.setdefault(k, []).append(op)
        for k in writes:
            if k in ("W", "Wk", "Wv", "Wf") and op.is_dma:
                self.multi_w.setdefault(k, []).append(op)
                continue
            w = self.last_w.get(k)
            if w is not None:
                deps.append(w)
            for r in self.readers.get(k, ()):
                if r is not op:
                    deps.append(r)
            self.last_w[k] = op
            self.readers[k] = []
        seen = set()
        for d in deps:
            if id(d) in seen or d is op:
                continue
            seen.add(id(d))
            if not d.is_dma and d.eng == op.eng and not op.is_dma:
                if op.eng == "pe":
                    continue
            op.deps.append(d)
        self.ops.append(op)
        return op

    def op(self, eng, emit, reads=(), writes=()):
        return self._add(Op(eng, emit), reads, writes)

    def dma(self, q, out, in_, reads=(), writes=()):
        op = Op(q, lambda e: e.dma_start(out=out, in_=in_), is_dma=True)
        return self._add(op, reads, writes)

    def idma(self, out, out_off, in_, in_off, reads=(), writes=(), bound=None):
        if bound is None:
            em = lambda e: e.indirect_dma_start(out=out, out_offset=out_off, in_=in_, in_offset=in_off)
        else:
            def em(e):
                key = (id(e), bound, len(self.ops_done))
                if key not in self.regcache:
                    self.regcache[key] = e.to_reg(bound)
                return e.indirect_dma_start(out=out, out_offset=out_off, in_=in_, in_offset=in_off,
                                            bounds_check=self.regcache[key], oob_is_err=False)
        op = Op("pool", em, is_dma=True)
        return self._add(op, reads, writes)

    def flush(self, name):
        nc = self.nc
        ops = self.ops
        for o in ops:
            for d in o.deps:
                if not d.is_dma:
                    d.sig = True
        for o in ops:
            if o.is_dma:
                q = o.eng
                i = self.dnext[q]
                self.dnext[q] = (i + 1) % NDMA
                o.pre = (self.dsem[q][i], self.dcnt[q][i])
                self.dcnt[q][i] += 16
                o.tok = (self.dsem[q][i], self.dcnt[q][i])
            elif o.sig:
                self.cnt[o.eng] += 1
                o.tok = (self.sem[o.eng], self.cnt[o.eng])
        per = {e: [o for o in ops if o.eng == e] for e in self.ENG}
        drain = []
        for q in ("sp", "pool"):
            for i in range(NDMA):
                drain.append((self.dsem[q][i], self.dcnt[q][i]))

        def run(eng_name, e):
            wd = self.waited[eng_name]

            def wait(tok):
                s, v = tok
                if v <= 0:
                    return
                if wd.get(id(s), 0) >= v:
                    return
                e.wait_ge(s, v)
                wd[id(s)] = v

            for o in per[eng_name]:
                if o.is_dma:
                    wait(o.pre)
                for d in o.deps:
                    wait(d.tok)
                ins = o.emit(e)
                if o.is_dma:
                    ins.then_inc(o.tok[0], 16)
                elif o.sig:
                    ins.then_inc(o.tok[0], 1)
            if eng_name == "sp":
                for t in drain:
                    wait(t)

        with nc.Block() as block:
            @block.sync
            def _(e):
                run("sp", e)

            @block.gpsimd
            def _(e):
                run("pool", e)

            @block.scalar
            def _(e):
                run("act", e)

            @block.vector
            def _(e):
                run("dve", e)

            @block.tensor
            def _(e):
                run("pe", e)
        self.ops_done.append(name)
        self.reset()


def mm(out, lhsT, rhs, start, stop):
    return lambda e: e.matmul(out, lhsT=lhsT, rhs=rhs, start=start, stop=stop)


def tr(out, in_, ident):
    return lambda e: e.transpose(out=out, in_=in_, identity=ident)


def act(out, in_, func, bias=None, scale=None, accum=None):
    kw = {}
    if bias is not None:
        kw["bias"] = bias
    if scale is not None:
        kw["scale"] = scale
    if accum is not None:
        kw["accum_out"] = accum
    return lambda e: e.activation(out=out, in_=in_, func=func, **kw)


def tt(out, a, b, op):
    return lambda e: e.tensor_tensor(out=out, in0=a, in1=b, op=op)


def ts(out, a, s1, s2, op0, op1=None):
    if op1 is None:
        return lambda e: e.tensor_scalar(out=out, in0=a, scalar1=s1, scalar2=None, op0=op0)
    return lambda e: e.tensor_scalar(out=out, in0=a, scalar1=s1, scalar2=s2, op0=op0, op1=op1)


def stt(out, a, s, b, op0, op1):
    return lambda e: e.scalar_tensor_tensor(out=out, in0=a, scalar=s, in1=b, op0=op0, op1=op1)


def cp(out, in_):
    return lambda e: e.tensor_copy(out=out, in_=in_)


def acp(out, in_):
    return lambda e: e.copy(out=out, in_=in_)


def rcp(out, in_):
    return lambda e: e.reciprocal(out=out, in_=in_)


def mset(ap, v):
    return lambda e: e.memset(ap, v)


def build_nc(debug=False):
    nc = bass.Bass("TRN2", target_bir_lowering=False)

    def din(name, shape, dt=F32):
        return nc.dram_tensor(name, list(shape), dt, kind="ExternalInput").ap()

    def dscr(name, shape, dt):
        kind = "ExternalOutput" if debug else "Internal"
        return nc.dram_tensor(name, list(shape), dt, kind=kind).ap()

    xs = din("xs", [SEQ, D])
    xq = din("xq", [NQ, D])
    w_in = din("w_in", [D, 5128])
    g_mix_b = din("g_mix_b", [128, D])
    g_moe_b = din("g_moe_b", [128, D])
    g_fin_b = din("g_fin_b", [128, D])
    b_fg = din("b_fg", [8, 1])
    b_gate = din("b_gate", [128, 16])
    lamv = din("lamv", [1, 256])
    g_sub = din("g_sub", [128, 1])
    w_pa = din("w_pa", [512, D])
    w_pb = din("w_pb", [512, D])
    w_o = din("w_o", [D, D])
    w_rt = din("w_rt", [D, 36])
    b_rt = din("b_rt", [128, 36])
    w13r = din("w13r", [32 * 128 * 4, 2048])
    w2r = din("w2r", [32 * 128 * 2, 2048])
    ltri = din("ltri", [128, 128])
    iot13 = din("iot13", [128, 4])
    iot2 = din("iot2", [128, 2])
    identd = din("identd", [128, 128])
    kaug_d = din("kaug_d", [4, 5, SEQ])
    qaug_d = din("qaug_d", [4, 5, NQ])
    kaug_f = din("kaug_f", [4, SEQ])
    neg4 = din("neg4", [4, NQ])
    mask_f = din("mask_f", [128, 4, 512])
    mask_d = din("mask_d", [4, 128, 4, 512])
    out = nc.dram_tensor("out", [NQ, D], F32, kind="ExternalOutput").ap()

    KT = dscr("KT", [16, 64, SEQ], BF16)
    QTs = dscr("QTs", [16, 64, NQ], BF16)
    Fs = dscr("Fs", [8, 3, SEQ], BF16)
    Fq = dscr("Fq", [8, 3, NQ], BF16)
    Vf = dscr("Vf", [64, 128, 520], BF16)
    Vd = dscr("Vd", [64, 128, 512], BF16)
    OAd = dscr("OAd", [8, 64, NQ], BF16)
    OBd = dscr("OBd", [4, 128, NQ], BF16)
    XM = dscr("XM", [NQ, D], F32)
    Xs = dscr("Xs", [NROWS, D], BF16)
    Yd = dscr("Yd", [NROWS, D], BF16)

    with contextlib.ExitStack() as gs:
        S = Sched(nc, gs)

        uid = [0]

        def sb(stack, name, shape, dt):
            uid[0] += 1
            return stack.enter_context(nc.sbuf_tensor("%s_%d" % (name, uid[0]), list(shape), dt))

        def ps(stack, name, shape, dt=F32):
            uid[0] += 1
            return stack.enter_context(nc.psum_tensor("%s_%d" % (name, uid[0]), list(shape), dt))

        ident = sb(gs, "ident", [128, 128], BF16)
        ones_f = sb(gs, "ones_f", [128, 128], F32)
        ones_c = sb(gs, "ones_c", [128, 1], BF16)
        gmix = sb(gs, "gmix", [128, D], F32)
        gmoe = sb(gs, "gmoe", [128, D], F32)
        gfin = sb(gs, "gfin", [128, D], F32)
        bfg = sb(gs, "bfg", [8, 1], F32)
        bgt = sb(gs, "bgt", [128, 16], F32)
        gs08 = sb(gs, "gs08", [128, 1], F32)
        brt = sb(gs, "brt", [128, 36], F32)
        lam_t = sb(gs, "lam_t", [1, 256], F32)
        lam_p = sb(gs, "lam_p", [1, 128], F32)
        lam_s = sb(gs, "lam_s", [1, 4], F32)
        neglam = sb(gs, "neglam", [1, 1], F32)
        A1_all = sb(gs, "A1_all", [128, 16, 32], F32)
        A2_all = sb(gs, "A2_all", [128, 16, 32], F32)
        WT = sb(gs, "WT", [128, 16, 2], F32)
        DESTi = sb(gs, "DESTi", [128, 16, 2], I32)
        idx13 = sb(gs, "idx13", [128, NBLK, 4], I32)
        idx2 = sb(gs, "idx2", [128, NBLK, 2], I32)
        junk = sb(gs, "junk", [128, D], BF16)

        S.dma("pool", ident[:], identd, writes=["ident"])
        S.dma("sp", gmix[:], g_mix_b, writes=["gmix"])
        S.dma("sp", gmoe[:], g_moe_b, writes=["gmoe"])
        S.dma("sp", gfin[:], g_fin_b, writes=["gfin"])
        S.dma("sp", bfg[:], b_fg, writes=["bfg"])
        S.dma("sp", bgt[:], b_gate, writes=["bgt"])
        S.dma("sp", gs08[:], g_sub, writes=["gs08"])
        S.dma("sp", brt[:], b_rt, writes=["brt"])
        S.dma("sp", lam_t[:], lamv, writes=["lam_t"])
        S.op("dve", mset(ones_f[:], 1.0), writes=["ones_f"])
        S.op("dve", mset(ones_c[:], 1.0), writes=["ones_c"])
        S.op("dve", ts(gs08[:], gs08[:], 1.0 - LAM_INIT, None, ALU.mult), reads=["gs08"], writes=["gs08"])
        S.op("dve", tt(lam_p[:, 0:64], lam_t[:, 0:64], lam_t[:, 64:128], ALU.mult), reads=["lam_t"], writes=["lam_p"])
        S.op("dve", tt(lam_p[:, 64:128], lam_t[:, 128:192], lam_t[:, 192:256], ALU.mult), reads=["lam_t", "lam_p"],
             writes=["lam_p"])
        S.op("dve", lambda e: e.reduce_sum(out=lam_s[:, 0:1], in_=lam_p[:, 0:64], axis=AX.X), reads=["lam_p"],
             writes=["lam_s"])
        S.op("dve", lambda e: e.reduce_sum(out=lam_s[:, 1:2], in_=lam_p[:, 64:128], axis=AX.X),
             reads=["lam_p", "lam_s"], writes=["lam_s"])
        S.op("act", act(lam_s[:, 2:4], lam_s[:, 0:2], AF.Exp), reads=["lam_s"], writes=["lam_s2"])
        S.op("dve", tt(neglam[:], lam_s[:, 3:4], lam_s[:, 2:3], ALU.subtract), reads=["lam_s2"], writes=["neglam"])
        S.op("dve", ts(neglam[:], neglam[:], -LAM_INIT, None, ALU.add), reads=["neglam"], writes=["neglam"])
        S.flush("setup")

        def norm_tile(xt_ap, xkey, gtile, gkey, hb, hbkey, st, stkey, out_dt_tile=None):
            S.op("act", act(junk[:], xt_ap, AF.Square, accum=st[:, 0:1]), reads=[xkey], writes=[stkey + "a", "junk"])
            S.op("act", act(st[:, 1:2], st[:, 0:1], AF.Sqrt, bias=EPS, scale=1.0 / D), reads=[stkey + "a"],
                 writes=[stkey + "b"])
            S.op("dve", rcp(st[:, 2:3], st[:, 1:2]), reads=[stkey + "b"], writes=[stkey + "c"])
            S.op("dve", stt(hb, xt_ap, st[:, 2:3], gtile[:], ALU.mult, ALU.mult), reads=[xkey, stkey + "c", gkey],
                 writes=[hbkey])

        with contextlib.ExitStack() as st1:
            Fc = sb(st1, "Fc", [8, SEQ], F32)
            ones8 = sb(st1, "ones8", [8, 512], F32)
            S.op("dve", mset(ones8[:], 1.0), writes=["ones8"])
            with contextlib.ExitStack() as p1:
                Wk = sb(p1, "Wk", [128, 8, 1024], BF16)
                Wv = sb(p1, "Wv", [128, 8, 1024], BF16)
                Wf = sb(p1, "Wf", [128, 8, 8], BF16)
                xt = [sb(p1, "xt%d" % i, [128, D], F32) for i in range(4)]
                hb = [sb(p1, "hb%d" % i, [128, D], BF16) for i in range(4)]
                stt_ = [sb(p1, "st%d" % i, [128, 4], F32) for i in range(4)]
                hT = [sb(p1, "hT%d" % i, [128, 8, 512], BF16) for i in range(2)]
                kst = [sb(p1, "kst%d" % i, [128, 512], BF16) for i in range(2)]
                vsf = [sb(p1, "vsf%d" % i, [128, 8, 65], BF16) for i in range(2)]
                vsd = [sb(p1, "vsd%d" % i, [128, 512], BF16) for i in range(2)]
                fs1 = sb(p1, "fs1", [8, 512], F32)
                lf = sb(p1, "lf", [8, 512], F32)
                fr = sb(p1, "fr", [8, 512], F32)
                fsp = [sb(p1, "fsp%d" % i, [8, 3, 512], BF16) for i in range(2)]
                ps_t = [ps(p1, "ps_t%d" % i, [128, 1024], BF16) for i in range(2)]
                ps_k = [ps(p1, "ps_k%d" % i, [128, 512]) for i in range(2)]
                ps_f = ps(p1, "ps_f", [128, 512])
                ps_v = [ps(p1, "ps_v%d" % i, [128, 512]) for i in range(3)]

                zt = sb(p1, "zt", [128, 4096], BF16)
                S.op("dve", mset(zt[:], 0.0), writes=["zt"])
                Xz = Xs.rearrange("(p a) d -> p (a d)", p=128)
                for zi in range(NROWS * D // 128 // 4096):
                    S.dma("sp", Xz[:, zi * 4096:(zi + 1) * 4096], zt[:], reads=["zt"])
                wsrc = w_in.rearrange("(c p) n -> p c n", p=128)
                for (dst, lo, wkey) in ((Wk[:, :, 0:512], C_FK, "Wk"), (Wk[:, :, 512:1024], C_DK, "Wk"),
                                        (Wv[:, :, 0:512], C_FV, "Wv"), (Wv[:, :, 512:1024], C_DV, "Wv")):
                    for hh in range(2):
                        S.dma("pool", dst[:, 4 * hh:4 * hh + 4, :], wsrc[:, 4 * hh:4 * hh + 4, lo:lo + 512],
                              writes=[wkey])
                S.dma("pool", Wf[:], wsrc[:, :, C_FF:C_FF + 8], writes=["Wf"])
                for i in range(2):
                    S.op("dve", mset(vsf[i][:], 1.0), writes=["vsf%d" % i])

                vstate = {"vcnt": 0}

                def norm_part(G, r):
                    T = 4 * G + r
                    S.dma("pool", xt[r][:], xs[T * 128:(T + 1) * 128, :], writes=["xt%d" % r])
                    norm_tile(xt[r][:], "xt%d" % r, gmix, "gmix", hb[r][:], "hb%d" % r, stt_[r], "st%d" % r)

                def tr_part(G, r):
                    g2 = G % 2
                    s2 = r % 2
                    for k in range(8):
                        S.op("pe", tr(ps_t[s2][:, k * 128:(k + 1) * 128], hb[r][:, k * 128:(k + 1) * 128], ident[:]),
                             reads=["hb%d" % r, "ident"], writes=["ps_t%d" % s2])
                    S.op("act" if r % 2 == 0 else "dve",
                         (acp if r % 2 == 0 else cp)(hT[g2][:, :, r * 128:(r + 1) * 128],
                                                     ps_t[s2][:, :].rearrange("p (k t) -> p k t", k=8)),
                         reads=["ps_t%d" % s2], writes=["hT%d_%d" % (g2, r)])

                def back(G):
                    g2 = G % 2
                    vcnt = vstate["vcnt"]
                    hkeys = ["hT%d_%d" % (g2, r) for r in range(4)]
                    for cg in range(8):
                        pk = cg % 2
                        for k in range(8):
                            S.op("pe", mm(ps_k[pk][:], Wk[:, k, cg * 128:(cg + 1) * 128], hT[g2][:, k, :], k == 0, k == 7),
                                 reads=hkeys + ["Wk"], writes=["ps_k%d" % pk])
                        S.op("act" if cg % 2 == 0 else "dve", (acp if cg % 2 == 0 else cp)(kst[pk][:], ps_k[pk][:]),
                             reads=["ps_k%d" % pk], writes=["kst%d" % pk])
                        S.dma("sp", KT[2 * cg, :, G * 512:(G + 1) * 512], kst[pk][0:64, :], reads=["kst%d" % pk])
                        S.dma("sp", KT[2 * cg + 1, :, G * 512:(G + 1) * 512], kst[pk][64:128, :], reads=["kst%d" % pk])
                        if cg in (5, 7) and G + 1 < 16:
                            tr_part(G + 1, (cg - 5) // 2)
                    for k in range(8):
                        S.op("pe", mm(ps_f[0:8, :], Wf[:, k, :], hT[g2][:, k, :], k == 0, k == 7),
                             reads=hkeys + ["Wf"], writes=["ps_f"])
                    S.op("act", act(fs1[:], ps_f[0:8, :], AF.Sigmoid, bias=bfg[:, 0:1]), reads=["ps_f", "bfg"],
                         writes=["fs1"])
                    S.op("act", act(lf[:], fs1[:], AF.Ln), reads=["fs1"], writes=["lf"])
                    init = 0.0 if G == 0 else Fc[:, G * 512 - 1:G * 512]
                    S.op("dve", (lambda o, d1, ini: (lambda e: e.tensor_tensor_scan(
                        out=o, data0=ones8[:], data1=d1, initial=ini, op0=ALU.mult, op1=ALU.add)))(
                        Fc[:, G * 512:(G + 1) * 512], lf[:], init),
                        reads=["lf", "ones8", "Fc"], writes=["Fc"])
                    fcur = Fc[:, G * 512:(G + 1) * 512]
                    fk = "fsp%d" % g2
                    S.op("dve", cp(fsp[g2][:, 0, :], fcur), reads=["Fc"], writes=[fk])
                    S.op("dve", tt(fr[:], fcur, fsp[g2][:, 0, :], ALU.subtract), reads=["Fc", fk], writes=["fr"])
                    S.op("dve", cp(fsp[g2][:, 1, :], fr[:]), reads=["fr", fk], writes=[fk])
                    S.op("dve", tt(fr[:], fr[:], fsp[g2][:, 1, :], ALU.subtract), reads=["fr", fk], writes=["fr"])
                    S.op("dve", cp(fsp[g2][:, 2, :], fr[:]), reads=["fr", fk], writes=[fk])
                    S.dma("sp", Fs[:, :, G * 512:(G + 1) * 512], fsp[g2][:], reads=[fk])
                    for r in range(4):
                        T = 4 * G + r
                        s2 = T % 2
                        for half in range(2):
                            pv = vcnt % 3
                            vcnt += 1
                            for k in range(8):
                                S.op("pe", mm(ps_v[pv][:], hT[g2][:, k, r * 128:(r + 1) * 128],
                                              Wv[:, k, half * 512:(half + 1) * 512], k == 0, k == 7),
                                     reads=[hkeys[r], "Wv"], writes=["ps_v%d" % pv])
                            if half == 0:
                                S.op("act", acp(vsf[s2][:, :, 0:64], ps_v[pv][:, :].rearrange("p (h d) -> p h d", h=8)),
                                     reads=["ps_v%d" % pv], writes=["vsf%d" % s2])
                                S.dma("sp", Vf[T], vsf[s2][:, :, :].rearrange("p h d -> p (h d)"), reads=["vsf%d" % s2])
                            else:
                                S.op("dve", cp(vsd[s2][:], ps_v[pv][:]), reads=["ps_v%d" % pv], writes=["vsd%d" % s2])
                                S.dma("sp", Vd[T], vsd[s2][:], reads=["vsd%d" % s2])
                        if r in (0, 1) and G + 1 < 16:
                            tr_part(G + 1, 2 + r)
                    vstate["vcnt"] = vcnt

                for r in range(4):
                    norm_part(0, r)
                for r in range(4):
                    tr_part(0, r)
                for G in range(16):
                    if G + 1 < 16:
                        for r in range(4):
                            norm_part(G + 1, r)
                    back(G)

                S.flush("p1")

            with contextlib.ExitStack() as p2:
                Wq = sb(p2, "Wq", [128, 8, 1024], BF16)
                xt = [sb(p2, "xt%d" % i, [128, D], F32) for i in range(4)]
                hb = [sb(p2, "hb%d" % i, [128, D], BF16) for i in range(4)]
                stt_ = [sb(p2, "st%d" % i, [128, 4], F32) for i in range(4)]
                hT = [sb(p2, "hT%d" % i, [128, 8, 512], BF16) for i in range(2)]
                qst = [sb(p2, "qst%d" % i, [128, 512], BF16) for i in range(2)]
                fqa = sb(p2, "fqa", [8, 512], F32)
                fr = sb(p2, "fr", [8, 512], F32)
                fsp = [sb(p2, "fsp%d" % i, [8, 3, 512], BF16) for i in range(2)]
                ps_t = [ps(p2, "ps_t%d" % i, [128, 1024], BF16) for i in range(2)]
                ps_k = [ps(p2, "ps_k%d" % i, [128, 512]) for i in range(2)]
                wsrc = w_in.rearrange("(c p) n -> p c n", p=128)
                for (dst, lo) in ((Wq[:, :, 0:512], C_FQ), (Wq[:, :, 512:1024], C_DQ)):
                    for hh in range(2):
                        S.dma("pool", dst[:, 4 * hh:4 * hh + 4, :], wsrc[:, 4 * hh:4 * hh + 4, lo:lo + 512],
                              writes=["W"])
                def norm_part2(m_, r):
                    T = 4 * m_ + r
                    S.dma("pool", xt[r][:], xq[T * 128:(T + 1) * 128, :], writes=["xt%d" % r])
                    norm_tile(xt[r][:], "xt%d" % r, gmix, "gmix", hb[r][:], "hb%d" % r, stt_[r], "st%d" % r)

                def tr_part2(m_, r):
                    g2_ = m_ % 2
                    s2 = r % 2
                    for k in range(8):
                        S.op("pe", tr(ps_t[s2][:, k * 128:(k + 1) * 128], hb[r][:, k * 128:(k + 1) * 128], ident[:]),
                             reads=["hb%d" % r, "ident"], writes=["ps_t%d" % s2])
                    S.op("act" if r % 2 == 0 else "dve",
                         (acp if r % 2 == 0 else cp)(hT[g2_][:, :, r * 128:(r + 1) * 128],
                                                     ps_t[s2][:, :].rearrange("p (k t) -> p k t", k=8)),
                         reads=["ps_t%d" % s2], writes=["hT%d_%d" % (g2_, r)])

                for r in range(4):
                    norm_part2(0, r)
                for r in range(4):
                    tr_part2(0, r)
                for m in range(4):
                    g2 = m % 2
                    if m + 1 < 4:
                        for r in range(4):
                            norm_part2(m + 1, r)
                    hkeys = ["hT%d_%d" % (g2, r) for r in range(4)]
                    for cg in range(8):
                        pk = cg % 2
                        for k in range(8):
                            S.op("pe", mm(ps_k[pk][:], Wq[:, k, cg * 128:(cg + 1) * 128], hT[g2][:, k, :], k == 0, k == 7),
                                 reads=hkeys + ["W"], writes=["ps_k%d" % pk])
                        S.op("act", lambda e, o=qst[pk][:], i=ps_k[pk][:]: e.mul(out=o, in_=i, mul=0.125),
                             reads=["ps_k%d" % pk], writes=["qst%d" % pk])
                        S.dma("sp", QTs[2 * cg, :, m * 512:(m + 1) * 512], qst[pk][0:64, :], reads=["qst%d" % pk])
                        S.dma("sp", QTs[2 * cg + 1, :, m * 512:(m + 1) * 512], qst[pk][64:128, :], reads=["qst%d" % pk])
                        if cg % 2 == 1 and m + 1 < 4:
                            tr_part2(m + 1, (cg - 1) // 2)
                    S.op("dve", cp(fqa[:], Fc[:, (4 * m + 3) * 512:(4 * m + 4) * 512]), writes=["fqa"])
                    fk = "fsp%d" % g2
                    S.op("dve", cp(fsp[g2][:, 0, :], fqa[:]), reads=["fqa"], writes=[fk])
                    S.op("dve", tt(fr[:], fqa[:], fsp[g2][:, 0, :], ALU.subtract), reads=["fqa", fk], writes=["fr"])
                    S.op("dve", cp(fsp[g2][:, 1, :], fr[:]), reads=["fr", fk], writes=[fk])
                    S.op("dve", tt(fr[:], fr[:], fsp[g2][:, 1, :], ALU.subtract), reads=["fr", fk], writes=["fr"])
                    S.op("dve", cp(fsp[g2][:, 2, :], fr[:]), reads=["fr", fk], writes=[fk])
                    S.dma("sp", Fq[:, :, m * 512:(m + 1) * 512], fsp[g2][:], reads=[fk])
                S.flush("p2")

        with contextlib.ExitStack() as p3:
            KTt = [sb(p3, "KTt%d" % i, [72, SEQ], BF16) for i in range(3)]
            QTt = [sb(p3, "QTt%d" % i, [72, NQ], BF16) for i in range(3)]
            Vall = sb(p3, "Vall", [128, 64, 520], BF16)
            mskf = sb(p3, "mskf", [128, 4, 512], BF16)
            mskd = sb(p3, "mskd", [128, 4, 4, 512], BF16)
            pt = [sb(p3, "pt%d" % i, [128, 512], BF16) for i in range(6)]
            pacc = [sb(p3, "pacc%d" % i, [128, 512], F32) for i in range(2)]
            pacc2 = [sb(p3, "pacc2_%d" % i, [128, 512], F32) for i in range(1)]
            accb = [sb(p3, "accb%d" % i, [128, 512], BF16) for i in range(3)]
            Os = sb(p3, "Os", [128, 512], F32)
            O1 = sb(p3, "O1", [128, 4, 512], F32)
            dn = sb(p3, "dn", [1, 512], F32)
            rdd = [sb(p3, "rdd%d" % i, [1, 512], F32) for i in range(2)]
            t1 = sb(p3, "t1", [128, 512], F32)
            t2 = sb(p3, "t2", [128, 512], F32)
            rd = t2
            rs = sb(p3, "rs", [128, 512], F32)
            Ost = [sb(p3, "Ost%d" % i, [128, 512], BF16) for i in range(1)]
            ps_s = [ps(p3, "ps_s%d" % i, [128, 512]) for i in range(4)]
            ps_o = ps(p3, "ps_o", [128, 512])
            ps_d = ps(p3, "ps_d", [128, 512])
            ps_b = [ps(p3, "ps_b%d" % i, [128, 512]) for i in range(2)]

            state = {"s": 0, "ost": 0, "pa": 0, "p": 0}

            def load_unit(u, slot, kaug_parts, qaug_parts):
                kk = "KTt%d" % slot
                qk = "QTt%d" % slot
                for c in range(4):
                    S.dma("sp", KTt[slot][0:64, c * 2048:(c + 1) * 2048], KT[u, :, c * 2048:(c + 1) * 2048],
                          writes=[kk + "_%d" % c])
                S.dma("sp", QTt[slot][0:64, :], QTs[u], writes=[qk + "_0"])
                for (q, lo, hi, src) in kaug_parts:
                    S.dma(q, KTt[slot][lo:hi, :], src, writes=[kk + "_a%d" % lo])
                for (q, lo, hi, src) in qaug_parts:
                    S.dma(q, QTt[slot][lo:hi, :], src, writes=[qk + "_a%d" % lo])
                kkeys = [kk + "_%d" % c for c in range(4)] + [kk + "_a%d" % lo for (_, lo, _, _) in kaug_parts]
                qkeys = [qk + "_0"] + [qk + "_a%d" % lo for (_, lo, _, _) in qaug_parts]
                return kkeys, qkeys

            def attn_pass(slot, R, kkeys, qkeys, m, mget, vget, M, sep_den, Odst, Okey, ddst, dkey, hook=None):
                n = 16 * m + 16
                slots = {}
                dve_idx = [[i_ for i_ in range(n) if i_ % 6 in (0, 2, 4) and (i_ // 2) % 2 == w_] for w_ in range(2)]
                pool_idx = [i_ for i_ in range(n) if i_ % 6 in (1, 3)]
                pa = state["pa"] % 2
                state["pa"] += 1

                def emit_S(i):
                    s = state["s"] % 4
                    state["s"] += 1
                    slots[i] = s
                    diag = i >= 16 * m + 12
                    S.op("pe", mm(ps_s[s][:], KTt[slot][0:R, i * 128:(i + 1) * 128], QTt[slot][0:R, m * 512:(m + 1) * 512],
                                  True, not diag), reads=kkeys + qkeys, writes=["ps_s%d" % s])
                    if diag:
                        S.op("pe", mm(ps_s[s][:], ident[:], mget(i - 16 * m - 12), False, True),
                             reads=["ident", "msk"], writes=["ps_s%d" % s])

                emit_S(0)
                emit_S(1)
                emit_S(2)
                for i in range(n):
                    s = slots[i]
                    p = state["p"] % 6
                    state["p"] += 1
                    S.op("act", act(pt[p][:], ps_s[s][:], AF.Exp), reads=["ps_s%d" % s], writes=["pt%d" % p])
                    if hook is not None:
                        for (hi_, hf_) in hook:
                            if hi_ == i:
                                hf_()
                    if i + 3 < n:
                        emit_S(i + 3)
                    S.op("pe", mm(ps_o[0:M, :], vget(i), pt[p][:], i == 0, i == n - 1), reads=["pt%d" % p, "Vall"],
                         writes=["ps_o"])
                    if sep_den:
                        if i % 6 == 5:
                            S.op("pe", mm(ps_d[0:1, :], ones_c[:], pt[p][:], i == 5, False),
                                 reads=["pt%d" % p, "ones_c"], writes=["ps_d"])
                        else:
                            if i % 6 in (1, 3):
                                eng_, acc_, ak_, lst_, ab_, abk_ = "pool", pacc2[0], "pacc2_0", pool_idx, accb[2], "accb2"
                            else:
                                w_ = (i // 2) % 2
                                eng_, acc_, ak_, lst_, ab_, abk_ = "dve", pacc[w_], "pacc%d" % w_, dve_idx[w_], accb[w_], "accb%d" % w_
                            if i == lst_[0]:
                                S.op(eng_, cp(acc_[:], pt[p][:]), reads=["pt%d" % p], writes=[ak_])
                            elif i == lst_[-1]:
                                S.op(eng_, tt(ab_[:], acc_[:], pt[p][:], ALU.add),
                                     reads=["pt%d" % p, ak_], writes=[abk_])
                            else:
                                S.op(eng_, tt(acc_[:], acc_[:], pt[p][:], ALU.add),
                                     reads=["pt%d" % p, ak_], writes=[ak_])
                S.op("act", acp(Odst, ps_o[0:M, :]), reads=["ps_o"], writes=[Okey])
                if sep_den:
                    for w_ in range(3):
                        S.op("pe", mm(ps_d[0:1, :], ones_c[:], accb[w_][:], False, w_ == 2),
                             reads=["accb%d" % w_, "ones_c"], writes=["ps_d"])
                    S.op("act", acp(ddst, ps_d[0:1, :]), reads=["ps_d"], writes=[dkey])

            S.dma("pool", mskf[:], mask_f, writes=["msk"])
            for h in range(4):
                S.dma("pool", mskd[:, h, :, :], mask_d[h], writes=["msk"])
            for c in range(4):
                S.dma("sp", Vall[:, 16 * c:16 * c + 16, :], Vf[16 * c:16 * c + 16].rearrange("t p f -> p t f"),
                      writes=["Vall"])

            def unit_loader(uu):
                slot = uu % 3
                if uu < 8:
                    return load_unit(uu, slot,
                                     [("sp", 64, 67, Fs[uu]), ("pool", 67, 71, kaug_f)],
                                     [("pool", 64, 68, neg4), ("sp", 68, 71, Fq[uu])])
                h = (uu - 8) // 2
                return load_unit(uu, slot, [("pool", 64, 69, kaug_d[h])], [("pool", 64, 69, qaug_d[h])])

            loaded = {0: unit_loader(0)}
            pend = {}
            for uu in range(16):
                slot = uu % 3
                if uu + 1 < 16:
                    loaded[uu + 1] = unit_loader(uu + 1)
                kkeys, qkeys = loaded[uu]
                if uu == 8:
                    for c in range(4):
                        S.dma("sp", Vall[:, 16 * c:16 * c + 16, 0:512],
                              Vd[16 * c:16 * c + 16].rearrange("t p f -> p t f"), writes=["Vall"])
                if uu < 8:
                    u = uu
                    for m in range(4):
                        attn_pass(slot, 71, kkeys, qkeys, m, lambda t: mskf[:, t, :],
                                  lambda i, u=u: Vall[:, i, u * 65:(u + 1) * 65], 65, False,
                                  Os[0:65, :], "Os", None, None, hook=pend.pop("f", None))

                        def post_fox_a():
                            S.op("dve", rcp(rd[64:65, :], Os[64:65, :]), reads=["Os"], writes=["t2"])

                        def post_fox_b(u=u, m=m):
                            S.op("pe", mm(ps_b[0][0:64, :], ones_f[64:65, 0:64], rd[64:65, :], True, True),
                                 reads=["t2", "ones_f"], writes=["ps_b0"])
                            o = 0
                            state["ost"] += 1
                            S.op("dve", tt(Ost[o][0:64, :], Os[0:64, :], ps_b[0][0:64, :], ALU.mult),
                                 reads=["Os", "ps_b0"], writes=["Ost%d" % o])
                            S.dma("sp", OAd[u, :, m * 512:(m + 1) * 512], Ost[o][0:64, :], reads=["Ost%d" % o])
                        pend["f"] = [(1, post_fox_a), (9, post_fox_b)]
                    continue
                h = (uu - 8) // 2
                half = (uu - 8) % 2
                for m in range(4):
                    attn_pass(slot, 69, kkeys, qkeys, m, lambda t, h=h: mskd[:, h, t, :],
                              lambda i, h=h: Vall[:, i, h * 128:(h + 1) * 128], 128, True,
                              Os[:], "Os", dn[:], "dn", hook=pend.pop("f", None))

                    def post_diff_a(half=half):
                        S.op("dve", rcp(rdd[half][:], dn[:]), reads=["dn"], writes=["rdd%d" % half])
                        if half == 1:
                            S.op("dve", ts(rdd[1][:], rdd[1][:], neglam[0:1, 0:1], None, ALU.mult),
                                 reads=["rdd1", "neglam"], writes=["rdd1"])

                    def post_diff_b(half=half, m=m):
                        S.op("pe", mm(ps_b[half][:], ones_f[0:1, :], rdd[half][:], True, True),
                             reads=["rdd%d" % half, "ones_f"], writes=["ps_b%d" % half])
                        if half == 0:
                            S.op("dve", tt(O1[:, m, :], Os[:], ps_b[0][:], ALU.mult), reads=["Os", "ps_b0"],
                                 writes=["O1_%d" % m])
                            return
                        S.op("dve", tt(t2[:], Os[:], ps_b[1][:], ALU.mult), reads=["Os", "ps_b1"], writes=["t2"])
                        S.op("dve", tt(t1[:], O1[:, m, :], t2[:], ALU.add), reads=["O1_%d" % m, "t2"], writes=["t1"])
                        S.op("act", act(t2[:], t1[:], AF.Square), reads=["t1"], writes=["t2"])

                    def post_diff_c(h=h, half=half, m=m):
                        if half == 0:
                            return
                        S.op("pe", mm(ps_b[0][:], ones_f[:], t2[:], True, True), reads=["t2", "ones_f"],
                             writes=["ps_b0"])
                        S.op("act", act(rs[:], ps_b[0][:], AF.Sqrt, bias=EPS, scale=1.0 / 128.0), reads=["ps_b0"],
                             writes=["rs"])
                        S.op("dve", rcp(rs[:], rs[:]), reads=["rs"], writes=["rs"])
                        o = 0
                        state["ost"] += 1
                        S.op("dve", stt(Ost[o][:], t1[:], gs08[:, 0:1], rs[:], ALU.mult, ALU.mult),
                             reads=["t1", "gs08", "rs"], writes=["Ost%d" % o])
                        S.dma("sp", OBd[h, :, m * 512:(m + 1) * 512], Ost[o][:], reads=["Ost%d" % o])
                    pend["f"] = [(1, post_diff_a), (8, post_diff_b), (14, post_diff_c)]
            for (_, hf_) in pend.pop("f"):
                hf_()
            S.flush("p3")

        st4 = gs.enter_context(contextlib.ExitStack())
        H2tok = sb(st4, "H2tok", [128, 16, D], BF16)
        with contextlib.ExitStack() as p4:
            Wpa = sb(p4, "Wpa", [64, 8, D], BF16)
            Wpb = sb(p4, "Wpb", [128, 4, D], BF16)
            Wg = sb(p4, "Wg", [128, 8, 2048], BF16)
            Wo = sb(p4, "Wo", [128, 8, D], BF16)
            Wr = sb(p4, "Wr", [128, 8, 36], BF16)
            xt4 = [sb(p4, "xt4_%d" % i, [128, D], F32) for i in range(2)]
            xr4 = [sb(p4, "xr4_%d" % i, [128, D], F32) for i in range(1)]
            hb = [sb(p4, "hb%d" % i, [128, D], BF16) for i in range(4)]
            stt_ = [sb(p4, "st%d" % i, [128, 4], F32) for i in range(3)]
            hT = [sb(p4, "hT%d" % i, [128, 8, 512], BF16) for i in range(2)]
            OAm = [sb(p4, "OAm%d" % i, [64, 8, 512], BF16) for i in range(1)]
            OBm = [sb(p4, "OBm%d" % i, [128, 4, 512], BF16) for i in range(1)]
            ga_s = sb(p4, "ga_s", [128, 512], F32)
            gb_s = sb(p4, "gb_s", [128, 512], F32)
            yT = sb(p4, "yT", [128, 8, 512], BF16)
            xm = [sb(p4, "xm%d" % i, [128, D], F32) for i in range(2)]
            h2T = [sb(p4, "h2T%d" % i, [128, 8, 512], BF16) for i in range(1)]
            lg = sb(p4, "lg", [128, 36], F32)
            rt = sb(p4, "rt", [128, 16], F32)
            pen = sb(p4, "pen", [128, 4], F32)
            elm = sb(p4, "elm", [128, 32], F32)
            m8 = sb(p4, "m8", [128, 8], F32)
            ge = sb(p4, "ge", [128, 4], F32)
            ps_t = ps(p4, "ps_t", [128, 1024], BF16)
            ps_a = ps(p4, "ps_a", [128, 512])
            ps_bb = ps(p4, "ps_bb", [128, 512])
            ps_za = ps(p4, "ps_za", [128, 512])
            ps_zb = ps(p4, "ps_zb", [128, 512])
            ps_o0 = ps(p4, "ps_o0", [128, 512])
            ps_o1 = ps(p4, "ps_o1", [128, 512])
            ps_r = ps(p4, "ps_r", [128, 512])

            for hh in range(2):
                S.dma("pool", Wpa[:, 4 * hh:4 * hh + 4, :],
                      w_pa.rearrange("(h p) n -> p h n", p=64)[:, 4 * hh:4 * hh + 4, :], writes=["W"])
            S.dma("pool", Wpb[:], w_pb.rearrange("(h p) n -> p h n", p=128), writes=["W"])
            wsrc = w_in.rearrange("(c p) n -> p c n", p=128)
            for cc in range(4):
                for hh in range(2):
                    S.dma("pool", Wg[:, 4 * hh:4 * hh + 4, cc * 512:(cc + 1) * 512],
                          wsrc[:, 4 * hh:4 * hh + 4, C_GA + cc * 512:C_GA + (cc + 1) * 512], writes=["W"])
            wo_src = w_o.rearrange("(c p) n -> p c n", p=128)
            for hh in range(2):
                S.dma("pool", Wo[:, 4 * hh:4 * hh + 4, :], wo_src[:, 4 * hh:4 * hh + 4, :], writes=["W"])
            S.dma("pool", Wr[:], w_rt.rearrange("(c p) n -> p c n", p=128), writes=["W"])

            def norm_part4(m_, r):
                T = 4 * m_ + r
                s3 = T % 3
                s2 = r % 2
                S.dma("pool", xt4[s2][:], xq[T * 128:(T + 1) * 128, :], writes=["xt4_%d" % s2])
                norm_tile(xt4[s2][:], "xt4_%d" % s2, gmix, "gmix", hb[r][:], "hb%d" % r, stt_[s3], "st%d" % s3)

            def tr_part4(m_, r):
                hs = m_ % 2
                for k in range(8):
                    S.op("pe", tr(ps_t[:, k * 128:(k + 1) * 128], hb[r][:, k * 128:(k + 1) * 128], ident[:]),
                         reads=["hb%d" % r, "ident"], writes=["ps_t"])
                S.op("act", acp(hT[hs][:, :, r * 128:(r + 1) * 128], ps_t[:, :].rearrange("p (k t) -> p k t", k=8)),
                     reads=["ps_t"], writes=["hT%d_%d" % (hs, r)])

            for r in range(4):
                norm_part4(0, r)
                tr_part4(0, r)
            for m in range(4):
                g2 = 0
                hs = m % 2
                S.dma("pool", OAm[g2][:], OAd[:, :, m * 512:(m + 1) * 512].rearrange("h p t -> p h t"),
                      writes=["OAm%d" % g2])
                S.dma("pool", OBm[g2][:], OBd[:, :, m * 512:(m + 1) * 512].rearrange("h p t -> p h t"),
                      writes=["OBm%d" % g2])
                hkeys = ["hT%d_%d" % (hs, r) for r in range(4)]
                for c in range(8):
                    if m + 1 < 4:
                        if c < 4:
                            norm_part4(m + 1, c)
                        else:
                            tr_part4(m + 1, c - 4)
                    if c % 2 == 0:
                        A_, Ak, B_, Bk, ZA, ZAk = ps_a, "ps_a", ps_bb, "ps_bb", ps_za, "ps_za"
                    else:
                        A_, Ak, B_, Bk, ZA, ZAk = ps_o0, "ps_o0", ps_o1, "ps_o1", ps_r, "ps_r"
                    for hh in range(8):
                        S.op("pe", mm(A_[:], Wpa[:, hh, c * 128:(c + 1) * 128], OAm[g2][:, hh, :], hh == 0, hh == 7),
                             reads=["W", "OAm%d" % g2], writes=[Ak])
                    for hh in range(4):
                        S.op("pe", mm(B_[:], Wpb[:, hh, c * 128:(c + 1) * 128], OBm[g2][:, hh, :], hh == 0, hh == 3),
                             reads=["W", "OBm%d" % g2], writes=[Bk])
                    for k in range(8):
                        S.op("pe", mm(ZA[:], Wg[:, k, c * 128:(c + 1) * 128], hT[hs][:, k, :], k == 0, k == 7),
                             reads=hkeys + ["W"], writes=[ZAk])
                    for k in range(8):
                        S.op("pe", mm(ps_zb[:], Wg[:, k, 1024 + c * 128:1024 + (c + 1) * 128], hT[hs][:, k, :], k == 0,
                                      k == 7), reads=hkeys + ["W"], writes=["ps_zb"])
                    S.op("act", act(ga_s[:], ZA[:], AF.Sigmoid, bias=bgt[:, c:c + 1]), reads=[ZAk, "bgt"],
                         writes=["ga_s"])
                    S.op("act", act(gb_s[:], ps_zb[:], AF.Sigmoid, bias=bgt[:, 8 + c:9 + c]), reads=["ps_zb", "bgt"],
                         writes=["gb_s"])
                    S.op("dve", tt(ga_s[:], ga_s[:], A_[:], ALU.mult), reads=["ga_s", Ak], writes=["ga_s"])
                    S.op("dve", tt(gb_s[:], gb_s[:], B_[:], ALU.mult), reads=["gb_s", Bk], writes=["gb_s"])
                    S.op("dve", tt(yT[:, c, :], ga_s[:], gb_s[:], ALU.add), reads=["ga_s", "gb_s"], writes=["yT_%d" % c])
                ykeys = ["yT_%d" % c for c in range(8)]

                def o_mm(r):
                    T_ = 4 * m + r
                    S.dma("pool", xr4[0][:], xq[T_ * 128:(T_ + 1) * 128, :], writes=["xr4_0"])
                    for half, pso, pk in ((0, ps_o0, "ps_o0"), (1, ps_o1, "ps_o1")):
                        for c in range(8):
                            S.op("pe", mm(pso[:], yT[:, c, r * 128:(r + 1) * 128], Wo[:, c, half * 512:(half + 1) * 512],
                                          c == 0, c == 7), reads=ykeys + ["W"], writes=[pk])

                def o_add(r):
                    T = 4 * m + r
                    s2 = T % 2
                    xk = "xr4_0"
                    for half, pso, pk in ((0, ps_o0, "ps_o0"), (1, ps_o1, "ps_o1")):
                        S.op("dve", tt(xm[s2][:, half * 512:(half + 1) * 512], pso[:],
                                       xr4[0][:, half * 512:(half + 1) * 512], ALU.add), reads=[pk, xk],
                             writes=["xm%d_%d" % (s2, half)])

                def o_rest(r):
                    T = 4 * m + r
                    s3 = T % 3
                    s2 = T % 2
                    xmk = ["xm%d_0" % s2, "xm%d_1" % s2]
                    S.dma("sp", XM[T * 128:(T + 1) * 128, :], xm[s2][:], reads=xmk)
                    S.op("act", act(junk[:], xm[s2][:], AF.Square, accum=stt_[s3][:, 0:1]), reads=xmk,
                         writes=["st%da" % s3, "junk"])
                    S.op("act", act(stt_[s3][:, 1:2], stt_[s3][:, 0:1], AF.Sqrt, bias=EPS, scale=1.0 / D),
                         reads=["st%da" % s3], writes=["st%db" % s3])
                    S.op("dve", rcp(stt_[s3][:, 2:3], stt_[s3][:, 1:2]), reads=["st%db" % s3], writes=["st%dc" % s3])
                    S.op("dve", stt(H2tok[:, T, :], xm[s2][:], stt_[s3][:, 2:3], gmoe[:], ALU.mult, ALU.mult),
                         reads=xmk + ["st%dc" % s3, "gmoe"], writes=["H2tok%d" % T])
                    for k in range(8):
                        S.op("pe", tr(ps_t[:, k * 128:(k + 1) * 128], H2tok[:, T, k * 128:(k + 1) * 128], ident[:]),
                             reads=["H2tok%d" % T, "ident"], writes=["ps_t"])
                    S.op("act", acp(h2T[g2][:, :, r * 128:(r + 1) * 128], ps_t[:, :].rearrange("p (k t) -> p k t", k=8)),
                         reads=["ps_t"], writes=["h2T%d_%d" % (g2, r)])
                    for k in range(8):
                        S.op("pe", mm(ps_r[:, 0:36], h2T[g2][:, k, r * 128:(r + 1) * 128], Wr[:, k, :], k == 0, k == 7),
                             reads=["h2T%d_%d" % (g2, r), "W"], writes=["ps_r"])
                    S.op("dve", tt(lg[:], ps_r[:, 0:36], brt[:], ALU.add), reads=["ps_r", "brt"], writes=["lg"])
                    S.op("dve", lambda e: e.reduce_max(out=rt[:, 0:1], in_=lg[:, 0:4], axis=AX.X), reads=["lg"],
                         writes=["rt0"])
                    S.op("dve", ts(rt[:, 1:2], rt[:, 0:1], -1.0, None, ALU.mult), reads=["rt0"], writes=["rt1"])
                    S.op("act", act(ge[:], lg[:, 0:4], AF.Exp, bias=rt[:, 1:2], accum=rt[:, 2:3]), reads=["lg", "rt1"],
                         writes=["ge", "rt2"])
                    S.op("dve", rcp(rt[:, 3:4], rt[:, 2:3]), reads=["rt2"], writes=["rt3"])
                    S.op("dve", ts(pen[:], lg[:, 0:4], rt[:, 0:1], 1.0e9, ALU.is_ge, ALU.mult), reads=["lg", "rt0"],
                         writes=["pen"])
                    S.op("dve", ts(pen[:], pen[:], -1.0e9, None, ALU.add), reads=["pen"], writes=["pen"])
                    for g in range(4):
                        S.op("dve", ts(elm[:, 8 * g:8 * g + 8], lg[:, 4 + 8 * g:12 + 8 * g], pen[:, g:g + 1], None, ALU.add),
                             reads=["lg", "pen", "elm"], writes=["elm"])
                    S.op("dve", lambda e: e.max(out=m8[:], in_=elm[:]), reads=["elm"], writes=["m8"])
                    S.op("dve", tt(rt[:, 4:5], m8[:, 0:1], m8[:, 1:2], ALU.subtract), reads=["m8"], writes=["rt4"])
                    S.op("act", act(rt[:, 5:6], rt[:, 4:5], AF.Sigmoid), reads=["rt4"], writes=["rt5"])
                    S.op("dve", ts(rt[:, 6:7], rt[:, 5:6], -1.0, 1.0, ALU.mult, ALU.add), reads=["rt5"], writes=["rt6"])
                    S.op("dve", ts(A1_all[:, T, :], elm[:], m8[:, 0:1], None, ALU.is_equal), reads=["elm", "m8"],
                         writes=["A1_%d" % T])
                    S.op("dve", ts(A2_all[:, T, :], elm[:], m8[:, 1:2], None, ALU.is_equal), reads=["elm", "m8"],
                         writes=["A2_%d" % T])
                    S.op("dve", tt(WT[:, T, 0:1], rt[:, 5:6], rt[:, 3:4], ALU.mult), reads=["rt5", "rt3"],
                         writes=["WT_%d" % T])
                    S.op("dve", tt(WT[:, T, 1:2], rt[:, 6:7], rt[:, 3:4], ALU.mult), reads=["rt6", "rt3", "WT_%d" % T],
                         writes=["WT_%d" % T])

                o_mm(0)
                for r in range(4):
                    o_add(r)
                    if r + 1 < 4:
                        o_mm(r + 1)
                    o_rest(r)
            S.flush("p4")

        IOA = bass.IndirectOffsetOnAxis
        with contextlib.ExitStack() as p4b:
            Ab = sb(p4b, "Ab", [128, 16, 32], BF16)
            Ltri = sb(p4b, "Ltri", [128, 128], BF16)
            onesb = sb(p4b, "onesb", [128, 128], BF16)
            ones32 = sb(p4b, "ones32", [128, 32], F32)
            cnt_s = sb(p4b, "cnt_s", [128, 32], F32)
            nbt = sb(p4b, "nbt", [128, 32], F32)
            padded = sb(p4b, "padded", [128, 32], F32)
            pend = sb(p4b, "pend", [128, 32], F32)
            pstart = sb(p4b, "pstart", [128, 32], F32)
            dst = sb(p4b, "dst", [128, 32], F32)
            tmp = sb(p4b, "tmp", [128, 32], F32)
            DESTf = sb(p4b, "DESTf", [128, 16, 2], F32)
            EBf = sb(p4b, "EBf", [128, NBLK], F32)
            EB1024 = sb(p4b, "EB1024", [128, NBLK], F32)
            EB512 = sb(p4b, "EB512", [128, NBLK], F32)
            cmpb = sb(p4b, "cmpb", [128, 32], F32)
            idx13f = sb(p4b, "idx13f", [128, NBLK, 4], F32)
            idx2f = sb(p4b, "idx2f", [128, NBLK, 2], F32)
            iot13t = sb(p4b, "iot13t", [128, 4], F32)
            iot2t = sb(p4b, "iot2t", [128, 2], F32)
            ps_pos = ps(p4b, "ps_pos", [128, 512])
            ps_cnt = ps(p4b, "ps_cnt", [128, 512])
            S.dma("pool", Ltri[:], ltri, writes=["Ltri"])
            S.dma("sp", iot13t[:], iot13, writes=["iot13t"])
            S.dma("sp", iot2t[:], iot2, writes=["iot2t"])
            S.op("dve", mset(onesb[:], 1.0), writes=["onesb"])
            S.op("dve", mset(ones32[:], 1.0), writes=["ones32"])
            for T in range(16):
                S.op("dve", tt(Ab[:, T, :], A1_all[:, T, :], A2_all[:, T, :], ALU.add), writes=["Ab%d" % T])
            for T in range(16):
                S.op("pe", mm(ps_cnt[:, 0:32], onesb[:], Ab[:, T, :], T == 0, T == 15), reads=["onesb", "Ab%d" % T],
                     writes=["ps_cnt"])
            S.op("dve", cp(cnt_s[:], ps_cnt[:, 0:32]), reads=["ps_cnt"], writes=["cnt_s"])
            S.op("dve", mset(nbt[:], 0.0), writes=["nbt"])
            for k in range(16):
                S.op("dve", stt(nbt[:], cnt_s[:], float(256 * k), nbt[:], ALU.is_gt, ALU.add), reads=["cnt_s", "nbt"],
                     writes=["nbt"])
            S.op("dve", ts(padded[:], nbt[:], 256.0, None, ALU.mult), reads=["nbt"], writes=["padded"])
            S.op("dve", lambda e: e.tensor_tensor_scan(out=pend[:], data0=ones32[:], data1=padded[:], initial=0.0,
                                                       op0=ALU.mult, op1=ALU.add), reads=["ones32", "padded"],
                 writes=["pend"])
            S.op("dve", tt(pstart[:], pend[:], padded[:], ALU.subtract), reads=["pend", "padded"], writes=["pstart"])
            for T in range(16):
                S.op("pe", mm(ps_pos[:, 0:32], Ltri[:], Ab[:, T, :], True, T == 0), reads=["Ltri", "Ab%d" % T],
                     writes=["ps_pos"])
                for T2 in range(T):
                    S.op("pe", mm(ps_pos[:, 0:32], onesb[:], Ab[:, T2, :], False, T2 == T - 1),
                         reads=["onesb", "Ab%d" % T2], writes=["ps_pos"])
                S.op("dve", tt(dst[:], ps_pos[:, 0:32], pstart[:], ALU.add), reads=["ps_pos", "pstart"], writes=["dst"])
                S.op("dve", tt(tmp[:], A1_all[:, T, :], dst[:], ALU.mult), reads=["dst"], writes=["tmp"])
                S.op("dve", lambda e, T=T: e.reduce_sum(out=DESTf[:, T, 0:1], in_=tmp[:], axis=AX.X), reads=["tmp"],
                     writes=["DESTf"])
                S.op("dve", tt(tmp[:], A2_all[:, T, :], dst[:], ALU.mult), reads=["dst", "tmp"], writes=["tmp"])
                S.op("dve", lambda e, T=T: e.reduce_sum(out=DESTf[:, T, 1:2], in_=tmp[:], axis=AX.X),
                     reads=["tmp", "DESTf"], writes=["DESTf"])
            S.op("dve", cp(DESTi[:], DESTf[:]), reads=["DESTf"], writes=["DESTi"])
            for T in range(16):
                for a_ in range(2):
                    S.idma(Xs, IOA(ap=DESTi[:, T, a_:a_ + 1], axis=0), H2tok[:, T, :], None, reads=["DESTi"])
            for b_ in range(NBLK):
                S.op("dve", ts(cmpb[:], pend[:], float(256 * b_), None, ALU.is_le), reads=["pend", "cmpb"],
                     writes=["cmpb"])
                S.op("dve", lambda e, b_=b_: e.reduce_sum(out=EBf[:, b_:b_ + 1], in_=cmpb[:], axis=AX.X),
                     reads=["cmpb", "EBf"], writes=["EBf"])
            S.op("dve", ts(EB1024[:], EBf[:], 512.0, None, ALU.mult), reads=["EBf"], writes=["EB1024"])
            S.op("dve", ts(EB512[:], EBf[:], 256.0, None, ALU.mult), reads=["EBf"], writes=["EB512"])
            for b_ in range(NBLK):
                S.op("dve", ts(idx13f[:, b_, :], iot13t[:], EB1024[:, b_:b_ + 1], None, ALU.add),
                     reads=["iot13t", "EB1024", "idx13f"], writes=["idx13f"])
                S.op("dve", ts(idx2f[:, b_, :], iot2t[:], EB512[:, b_:b_ + 1], None, ALU.add),
                     reads=["iot2t", "EB512", "idx2f"], writes=["idx2f"])
            S.op("dve", cp(idx13[:], idx13f[:]), reads=["idx13f"], writes=["idx13"])
            S.op("dve", cp(idx2[:], idx2f[:]), reads=["idx2f"], writes=["idx2"])
            S.flush("p4b")
        st4.close()

        with contextlib.ExitStack() as p5:
            W13 = [sb(p5, "W13_%d" % i, [128, 8, 1024], BF16) for i in range(2)]
            W2s = [sb(p5, "W2s_%d" % i, [128, 4, D], BF16) for i in range(2)]
            Xb = [sb(p5, "Xb%d" % i, [128, 2, D], BF16) for i in range(2)]
            xT = [sb(p5, "xT%d" % i, [128, 8, 256], BF16) for i in range(2)]
            sl = [sb(p5, "sl%d" % i, [128, 512], F32) for i in range(2)]
            GT = [sb(p5, "GT%d" % i, [128, 1024], BF16) for i in range(2)]
            Ys = [sb(p5, "Ys%d" % i, [128, 2, D], BF16) for i in range(2)]
            ps_t = [ps(p5, "ps_t%d" % i, [128, 1024], BF16) for i in range(2)]
            ps_h1 = [ps(p5, "ps_h1%d" % i, [128, 512]) for i in range(2)]
            ps_h3 = [ps(p5, "ps_h3%d" % i, [128, 512]) for i in range(2)]
            ps_y = [ps(p5, "ps_y%d" % i, [128, 512]) for i in range(2)]

            def issue(b_):
                sl_ = b_ % 2
                for c in range(4):
                    S.idma(W13[sl_][:, 2 * c:2 * c + 2, :].rearrange("p a n -> p (a n)"), None, w13r,
                           IOA(ap=idx13[:, b_, c:c + 1], axis=0),
                           writes=["W13_%d_%d" % (sl_, c)], bound=32 * 128 * 4 - 1)
                for c in range(2):
                    S.idma(W2s[sl_][:, 2 * c:2 * c + 2, :].rearrange("p a n -> p (a n)"), None, w2r,
                           IOA(ap=idx2[:, b_, c:c + 1], axis=0),
                           writes=["W2s_%d_%d" % (sl_, c)], bound=32 * 128 * 2 - 1)
                S.dma("sp", Xb[sl_][:], Xs[b_ * 256:(b_ + 1) * 256, :].rearrange("(t p) d -> p t d", p=128),
                      writes=["Xb%d" % sl_])

            issue(0)
            for b_ in range(NBLK):
                sl_ = b_ % 2
                if b_ + 1 < NBLK:
                    issue(b_ + 1)
                wk13 = ["W13_%d_%d" % (sl_, c) for c in range(4)]
                wk2 = ["W2s_%d_%d" % (sl_, c) for c in range(2)]
                for half in range(2):
                    for k in range(8):
                        S.op("pe", tr(ps_t[half][:, k * 128:(k + 1) * 128], Xb[sl_][:, half, k * 128:(k + 1) * 128],
                                      ident[:]), reads=["Xb%d" % sl_, "ident"], writes=["ps_t%d" % half])
                    S.op("act" if half == 0 else "dve",
                         (acp if half == 0 else cp)(xT[sl_][:, :, half * 128:(half + 1) * 128],
                                                    ps_t[half][:, :].rearrange("p (k t) -> p k t", k=8)),
                         reads=["ps_t%d" % half], writes=["xT%d_%d" % (sl_, half)])
                xk = ["xT%d_0" % sl_, "xT%d_1" % sl_]
                for bank in range(2):
                    for f2 in range(2):
                        fc = 2 * bank + f2
                        for k in range(8):
                            S.op("pe", mm(ps_h1[bank][:, f2 * 256:(f2 + 1) * 256], W13[sl_][:, k, fc * 128:(fc + 1) * 128],
                                          xT[sl_][:, k, :], k == 0, k == 7), reads=wk13 + xk, writes=["ps_h1%d" % bank])
                        for k in range(8):
                            S.op("pe", mm(ps_h3[bank][:, f2 * 256:(f2 + 1) * 256],
                                          W13[sl_][:, k, 512 + fc * 128:512 + (fc + 1) * 128],
                                          xT[sl_][:, k, :], k == 0, k == 7), reads=wk13 + xk, writes=["ps_h3%d" % bank])
                    S.op("act", act(sl[bank][:], ps_h1[bank][:], AF.Silu), reads=["ps_h1%d" % bank],
                         writes=["sl%d" % bank])
                    S.op("dve", tt(GT[sl_][:, bank * 512:(bank + 1) * 512], sl[bank][:], ps_h3[bank][:], ALU.mult),
                         reads=["sl%d" % bank, "ps_h3%d" % bank], writes=["GT%d_%d" % (sl_, bank)])
                gk = ["GT%d_0" % sl_, "GT%d_1" % sl_]
                for half in range(2):
                    for nh in range(2):
                        for fc in range(4):
                            S.op("pe", mm(ps_y[nh][:], GT[sl_][:, fc * 256 + half * 128:fc * 256 + (half + 1) * 128],
                                          W2s[sl_][:, fc, nh * 512:(nh + 1) * 512], fc == 0, fc == 3),
                                 reads=gk + wk2, writes=["ps_y%d" % nh])
                        S.op("act" if nh == 0 else "dve",
                             (acp if nh == 0 else cp)(Ys[sl_][:, half, nh * 512:(nh + 1) * 512], ps_y[nh][:]),
                             reads=["ps_y%d" % nh], writes=["Ys%d_%d_%d" % (sl_, half, nh)])
                S.dma("sp", Yd[b_ * 256:(b_ + 1) * 256, :].rearrange("(t p) d -> p t d", p=128), Ys[sl_][:],
                      reads=["Ys%d_%d_%d" % (sl_, h_, n_) for h_ in range(2) for n_ in range(2)])
            S.flush("p5")

        with contextlib.ExitStack() as p6:
            Y1 = [sb(p6, "Y1_%d" % i, [128, D], BF16) for i in range(2)]
            Y2 = [sb(p6, "Y2_%d" % i, [128, D], BF16) for i in range(2)]
            xmt = [sb(p6, "xmt%d" % i, [128, D], F32) for i in range(2)]
            stt_ = [sb(p6, "st%d" % i, [128, 4], F32) for i in range(2)]
            ot = [sb(p6, "ot%d" % i, [128, D], F32) for i in range(2)]
            def pre6(T):
                s2 = T % 2
                S.idma(Y1[s2][:], None, Yd, IOA(ap=DESTi[:, T, 0:1], axis=0), writes=["Y1_%d" % s2])
                S.idma(Y2[s2][:], None, Yd, IOA(ap=DESTi[:, T, 1:2], axis=0), writes=["Y2_%d" % s2])
                S.dma("sp", xmt[s2][:], XM[T * 128:(T + 1) * 128, :], writes=["xmt%d" % s2])

            pre6(0)
            for T in range(16):
                s2 = T % 2
                if T + 1 < 16:
                    pre6(T + 1)
                S.op("dve", stt(xmt[s2][:], Y1[s2][:], WT[:, T, 0:1], xmt[s2][:], ALU.mult, ALU.add),
                     reads=["Y1_%d" % s2, "xmt%d" % s2], writes=["xmt%d" % s2])
                S.op("dve", stt(xmt[s2][:], Y2[s2][:], WT[:, T, 1:2], xmt[s2][:], ALU.mult, ALU.add),
                     reads=["Y2_%d" % s2, "xmt%d" % s2], writes=["xmt%d" % s2])
                S.op("act", act(junk[:], xmt[s2][:], AF.Square, accum=stt_[s2][:, 0:1]), reads=["xmt%d" % s2],
                     writes=["st%da" % s2, "junk"])
                S.op("act", act(stt_[s2][:, 1:2], stt_[s2][:, 0:1], AF.Sqrt, bias=EPS, scale=1.0 / D),
                     reads=["st%da" % s2], writes=["st%db" % s2])
                S.op("dve", rcp(stt_[s2][:, 2:3], stt_[s2][:, 1:2]), reads=["st%db" % s2], writes=["st%dc" % s2])
                S.op("dve", stt(ot[s2][:], xmt[s2][:], stt_[s2][:, 2:3], gfin[:], ALU.mult, ALU.mult),
                     reads=["xmt%d" % s2, "st%dc" % s2, "gfin"], writes=["ot%d" % s2])
                S.dma("sp", out[T * 128:(T + 1) * 128, :], ot[s2][:], reads=["ot%d" % s2])
            S.flush("p6")
    return nc


def _bf16_round(a):
    a = np.ascontiguousarray(a, dtype=np.float32)
    u = a.view(np.uint32).astype(np.uint64)
    u = (u + 0x7FFF + ((u >> 16) & 1)) & 0xFFFF0000
    return u.astype(np.uint32).view(np.float32)


def _consts():
    slopes = [2.0 ** (-2.0 * (h + 1)) for h in range(4)]
    kpos = np.arange(SEQ, dtype=np.float64)
    qpos = np.concatenate([np.arange((4 * m + 3) * 512, (4 * m + 4) * 512) for m in range(4)]).astype(np.float64)
    kaug = np.zeros((4, 5, SEQ), np.float32)
    qaug = np.zeros((4, 5, NQ), np.float32)
    for h, s in enumerate(slopes):
        v = (s * kpos).astype(np.float32)
        hi = _bf16_round(v)
        kaug[h, 0] = 1.0
        kaug[h, 1] = 1.0
        kaug[h, 2] = hi
        kaug[h, 3] = v - hi
        vq = (-s * qpos).astype(np.float32)
        hq = _bf16_round(vq)
        qaug[h, 0] = hq
        qaug[h, 1] = vq - hq
        qaug[h, 2] = 1.0
        qaug[h, 3] = 1.0
        qaug[h, 4] = -1.0
    ki = np.arange(128)[:, None, None]
    t = np.arange(4)[None, :, None]
    qi = np.arange(512)[None, None, :]
    krel = 128 * t + ki
    qrel = qi + 0 * krel
    mask_f = np.where(krel <= qrel, 0.0, NEG).astype(np.float32)
    mask_d = np.zeros((4, 128, 4, 512), np.float32)
    allowed = (krel // 64) <= (qrel // 64)
    fut = np.maximum(krel - qrel, 0).astype(np.float64)
    for h, s in enumerate(slopes):
        mask_d[h] = np.where(allowed, -2.0 * s * fut, NEG).astype(np.float32)
    return kaug, qaug, mask_f, mask_d


def _in_maps(inp):
    f = np.float32
    x = np.asarray(inp["x"], f)
    w_in = np.ascontiguousarray(np.asarray(inp["w_in"], f)[0])
    tile128 = lambda v: np.ascontiguousarray(np.broadcast_to(np.asarray(v, f).reshape(1, -1), (128, np.asarray(v).size)))
    shared = {
        "w_in": w_in,
        "g_mix_b": tile128(inp["g_mix"][0]),
        "g_moe_b": tile128(inp["g_moe"][0]),
        "g_fin_b": tile128(inp["g_final"]),
        "b_fg": np.ascontiguousarray(np.asarray(inp["b_fgate"], f)[0].reshape(8, 1)),
        "b_gate": np.ascontiguousarray(np.asarray(inp["b_gate"], f)[0].reshape(16, 128).T),
        "lamv": np.ascontiguousarray(np.concatenate([np.asarray(inp[k], f)[0] for k in
                                                     ("lam_q1", "lam_k1", "lam_q2", "lam_k2")]).reshape(1, 256)),
        "g_sub": np.ascontiguousarray(np.asarray(inp["g_subln"], f)[0].reshape(128, 1)),
        "w_pa": np.ascontiguousarray(np.asarray(inp["w_pa"], f)[0]),
        "w_pb": np.ascontiguousarray(np.asarray(inp["w_pb"], f)[0]),
        "w_o": np.ascontiguousarray(np.asarray(inp["w_o"], f)[0]),
        "w_rt": np.ascontiguousarray(np.concatenate([np.asarray(inp["w_group"], f)[0],
                                                     np.asarray(inp["w_expert"], f)[0]], axis=1)),
        "b_rt": tile128(np.concatenate([np.asarray(inp["b_group"], f)[0].reshape(-1),
                                        np.asarray(inp["b_expert"], f)[0].reshape(-1)])),
        "w13r": np.ascontiguousarray(np.concatenate([np.asarray(inp["w1"], f)[0], np.asarray(inp["w3"], f)[0]],
                                                    axis=-1).reshape(32, 8, 128, 1024).transpose(0, 2, 1, 3)
                                     ).reshape(32 * 128 * 4, 2048),
        "w2r": np.ascontiguousarray(np.asarray(inp["w2"], f)[0].reshape(32, 4, 128, 1024).transpose(0, 2, 1, 3)
                                    ).reshape(32 * 128 * 2, 2048),
        "ltri": np.triu(np.ones((128, 128), f), 1),
        "iot13": (np.arange(128, dtype=f)[:, None] * 4 + np.arange(4, dtype=f)[None, :]),
        "iot2": (np.arange(128, dtype=f)[:, None] * 2 + np.arange(2, dtype=f)[None, :]),
        "identd": np.eye(128, dtype=f),
        "neg4": -np.ones((4, NQ), f),
    }
    kaug, qaug, mask_f, mask_d = _consts()
    shared["qaug_d"] = qaug
    shared["mask_f"] = mask_f
    shared["mask_d"] = mask_d
    maps = []
    for core in range(8):
        b, j = core // 4, core % 4
        shift = (3 - j) * 512
        d = dict(shared)
        xsc = np.zeros((SEQ, D), f)
        xsc[shift:] = x[b, :SEQ - shift]
        d["xs"] = xsc
        d["xq"] = np.ascontiguousarray(np.concatenate([x[b, (4 * m + j) * 512:(4 * m + j + 1) * 512] for m in range(4)], 0))
        pad = np.zeros((SEQ,), f)
        pad[:shift] = 1.0e30
        kd = kaug.copy()
        kd[:, 4, :] = pad
        d["kaug_d"] = kd
        kf = np.ones((4, SEQ), f)
        kf[0] = pad
        d["kaug_f"] = kf
        maps.append(d)
    return maps


_NC = {}


def kernel(**inputs):
    if "nc" not in _NC:
        _NC["nc"] = build_nc()
    nc = _NC["nc"]
    maps = _in_maps(inputs)
    res = run_bass_kernel_spmd(nc, maps, core_ids=list(range(8)))
    outp = np.zeros((2, SEQ, D), np.float32)
    for core in range(8):
        b, j = core // 4, core % 4
        o = np.asarray(res.results[core]["out"], np.float32)
        for m in range(4):
            outp[b, (4 * m + j) * 512:(4 * m + j + 1) * 512] = o[m * 512:(m + 1) * 512]
    return outp
```

```python
import contextlib
import numpy as np
import concourse.bass as bass
import concourse.mybir as mybir
from concourse.bass_utils import run_bass_kernel_spmd

F32 = mybir.dt.float32
I32 = mybir.dt.int32
BF16 = mybir.dt.bfloat16
AF = mybir.ActivationFunctionType
ALU = mybir.AluOpType
AX = mybir.AxisListType

D = 1024
SEQ = 8192
NQ = 2048
EPS = 1e-6
NEG = -1.0e30
LAM_INIT = 0.2
C_FQ, C_FK, C_FV, C_FF, C_DQ, C_DK, C_DV, C_GA, C_GB = 0, 512, 1024, 1536, 1544, 2056, 2568, 3080, 4104
NDMA = 8
NBLK = 48
NROWS = NBLK * 256


class Op:
    __slots__ = ("eng", "emit", "deps", "sig", "tok", "is_dma", "pre")

    def __init__(self, eng, emit, is_dma=False):
        self.eng = eng
        self.emit = emit
        self.deps = []
        self.sig = False
        self.tok = None
        self.is_dma = is_dma
        self.pre = None


class Sched:
    ENG = ("pe", "act", "dve", "pool", "sp")

    def __init__(self, nc, stack):
        self.nc = nc
        self.sem = {e: stack.enter_context(nc.semaphore("s_" + e)) for e in ("pe", "act", "dve", "pool")}
        self.cnt = {e: 0 for e in self.sem}
        self.dsem = {q: [stack.enter_context(nc.semaphore("d_%s%d" % (q, i))) for i in range(NDMA)]
                     for q in ("sp", "pool")}
        self.dcnt = {q: [0] * NDMA for q in ("sp", "pool")}
        self.dnext = {q: 0 for q in ("sp", "pool")}
        self.waited = {e: {} for e in self.ENG}
        self.regcache = {}
        self.ops_done = []
        self.reset()

    def reset(self):
        self.ops = []
        self.last_w = {}
        self.readers = {}
        self.multi_w = {}

    def _add(self, op, reads, writes):
        deps = []
        for k in reads:
            if k in ("W", "Wk", "Wv", "Wf"):
                deps.extend(self.multi_w.get(k, ()))
                continue
            w = self.last_w.get(k)
            if w is not None:
                deps.append(w)
            self.readers.setdefault(k, []).append(op)
        for k in writes:
            if k in ("W", "Wk", "Wv", "Wf") and op.is_dma:
                self.multi_w.setdefault(k, []).append(op)
                continue
            w = self.last_w.get(k)
            if w is not None:
                deps.append(w)
            for r in self.readers.get(k, ()):
                if r is not op:
                    deps.append(r)
            self.last_w[k] = op
            self.readers[k] = []
        seen = set()
        for d in deps:
            if id(d) in seen or d is op:
                continue
            seen.add(id(d))
            if not d.is_dma and d.eng == op.eng and not op.is_dma:
                if op.eng == "pe":
                    continue
            op.deps.append(d)
        self.ops.append(op)
        return op

    def op(self, eng, emit, reads=(), writes=()):
        return self._add(Op(eng, emit), reads, writes)

    def dma(self, q, out, in_, reads=(), writes=()):
        op = Op(q, lambda e: e.dma_start(out=out, in_=in_), is_dma=True)
        return self._add(op, reads, writes)

    def idma(self, out, out_off, in_, in_off, reads=(), writes=(), bound=None):
        if bound is None:
            em = lambda e: e.indirect_dma_start(out=out, out_offset=out_off, in_=in_, in_offset=in_off)
        else:
            def em(e):
                key = (id(e), bound, len(self.ops_done))
                if key not in self.regcache:
                    self.regcache[key] = e.to_reg(bound)
                return e.indirect_dma_start(out=out, out_offset=out_off, in_=in_, in_offset=in_off,
                                            bounds_check=self.regcache[key], oob_is_err=False)
        op = Op("pool", em, is_dma=True)
        return self._add(op, reads, writes)

    def flush(self, name):
        nc = self.nc
        ops = self.ops
        for o in ops:
            for d in o.deps:
                if not d.is_dma:
                    d.sig = True
        for o in ops:
            if o.is_dma:
                q = o.eng
                i = self.dnext[q]
                self.dnext[q] = (i + 1) % NDMA
                o.pre = (self.dsem[q][i], self.dcnt[q][i])
                self.dcnt[q][i] += 16
                o.tok = (self.dsem[q][i], self.dcnt[q][i])
            elif o.sig:
                self.cnt[o.eng] += 1
                o.tok = (self.sem[o.eng], self.cnt[o.eng])
        per = {e: [o for o in ops if o.eng == e] for e in self.ENG}
        drain = []
        for q in ("sp", "pool"):
            for i in range(NDMA):
                drain.append((self.dsem[q][i], self.dcnt[q][i]))

        def run(eng_name, e):
            wd = self.waited[eng_name]

            def wait(tok):
                s, v = tok
                if v <= 0:
                    return
                if wd.get(id(s), 0) >= v:
                    return
                e.wait_ge(s, v)
                wd[id(s)] = v

            for o in per[eng_name]:
                if o.is_dma:
                    wait(o.pre)
                for d in o.deps:
                    wait(d.tok)
                ins = o.emit(e)
                if o.is_dma:
                    ins.then_inc(o.tok[0], 16)
                elif o.sig:
                    ins.then_inc(o.tok[0], 1)
            if eng_name == "sp":
                for t in drain:
                    wait(t)

        with nc.Block() as block:
            @block.sync
            def _(e):
                run("sp", e)

            @block.gpsimd
            def _(e):
                run("pool", e)

            @block.scalar
            def _(e):
                run("act", e)

            @block.vector
            def _(e):
                run("dve", e)

            @block.tensor
            def _(e):
                run("pe", e)
        self.ops_done.append(name)
        self.reset()


def mm(out, lhsT, rhs, start, stop):
    return lambda e: e.matmul(out, lhsT=lhsT, rhs=rhs, start=start, stop=stop)


def tr(out, in_, ident):
    return lambda e: e.transpose(out=out, in_=in_, identity=ident)


def act(out, in_, func, bias=None, scale=None, accum=None):
    kw = {}
    if bias is not None:
        kw["bias"] = bias
    if scale is not None:
        kw["scale"] = scale
    if accum is not None:
        kw["accum_out"] = accum
    return lambda e: e.activation(out=out, in_=in_, func=func, **kw)


def tt(out, a, b, op):
    return lambda e: e.tensor_tensor(out=out, in0=a, in1=b, op=op)


def ts(out, a, s1, s2, op0, op1=None):
    if op1 is None:
        return lambda e: e.tensor_scalar(out=out, in0=a, scalar1=s1, scalar2=None, op0=op0)
    return lambda e: e.tensor_scalar(out=out, in0=a, scalar1=s1, scalar2=s2, op0=op0, op1=op1)


def stt(out, a, s, b, op0, op1):
    return lambda e: e.scalar_tensor_tensor(out=out, in0=a, scalar=s, in1=b, op0=op0, op1=op1)


def cp(out, in_):
    return lambda e: e.tensor_copy(out=out, in_=in_)


def acp(out, in_):
    return lambda e: e.copy(out=out, in_=in_)


def rcp(out, in_):
    return lambda e: e.reciprocal(out=out, in_=in_)


def mset(ap, v):
    return lambda e: e.memset(ap, v)


def build_nc(debug=False):
    nc = bass.Bass("TRN2", target_bir_lowering=False)

    def din(name, shape, dt=F32):
        return nc.dram_tensor(name, list(shape), dt, kind="ExternalInput").ap()

    def dscr(name, shape, dt):
        kind = "ExternalOutput" if debug else "Internal"
        return nc.dram_tensor(name, list(shape), dt, kind=kind).ap()

    xs = din("xs", [SEQ, D])
    xq = din("xq", [NQ, D])
    w_in = din("w_in", [D, 5128])
    g_mix_b = din("g_mix_b", [128, D])
    g_moe_b = din("g_moe_b", [128, D])
    g_fin_b = din("g_fin_b", [128, D])
    b_fg = din("b_fg", [8, 1])
    b_gate = din("b_gate", [128, 16])
    lamv = din("lamv", [1, 256])
    g_sub = din("g_sub", [128, 1])
    w_pa = din("w_pa", [512, D])
    w_pb = din("w_pb", [512, D])
    w_o = din("w_o", [D, D])
    w_rt = din("w_rt", [D, 36])
    b_rt = din("b_rt", [128, 36])
    w13r = din("w13r", [32 * 128 * 4, 2048])
    w2r = din("w2r", [32 * 128 * 2, 2048])
    ltri = din("ltri", [128, 128])
    iot13 = din("iot13", [128, 4])
    iot2 = din("iot2", [128, 2])
    identd = din("identd", [128, 128])
    kaug_d = din("kaug_d", [4, 5, SEQ])
    qaug_d = din("qaug_d", [4, 5, NQ])
    kaug_f = din("kaug_f", [4, SEQ])
    neg4 = din("neg4", [4, NQ])
    mask_f = din("mask_f", [128, 4, 512])
    mask_d = din("mask_d", [4, 128, 4, 512])
    out = nc.dram_tensor("out", [NQ, D], F32, kind="ExternalOutput").ap()

    KT = dscr("KT", [16, 64, SEQ], BF16)
    QTs = dscr("QTs", [16, 64, NQ], BF16)
    Fs = dscr("Fs", [8, 3, SEQ], BF16)
    Fq = dscr("Fq", [8, 3, NQ], BF16)
    Vf = dscr("Vf", [64, 128, 520], BF16)
    Vd = dscr("Vd", [64, 128, 512], BF16)
    OAd = dscr("OAd", [8, 64, NQ], BF16)
    OBd = dscr("OBd", [4, 128, NQ], BF16)
    XM = dscr("XM", [NQ, D], F32)
    Xs = dscr("Xs", [NROWS, D], BF16)
    Yd = dscr("Yd", [NROWS, D], BF16)

    with contextlib.ExitStack() as gs:
        S = Sched(nc, gs)

        uid = [0]

        def sb(stack, name, shape, dt):
            uid[0] += 1
            return stack.enter_context(nc.sbuf_tensor("%s_%d" % (name, uid[0]), list(shape), dt))

        def ps(stack, name, shape, dt=F32):
            uid[0] += 1
            return stack.enter_context(nc.psum_tensor("%s_%d" % (name, uid[0]), list(shape), dt))

        ident = sb(gs, "ident", [128, 128], BF16)
        ones_f = sb(gs, "ones_f", [128, 128], F32)
        ones_c = sb(gs, "ones_c", [128, 1], BF16)
        gmix = sb(gs, "gmix", [128, D], F32)
        gmoe = sb(gs, "gmoe", [128, D], F32)
        gfin = sb(gs, "gfin", [128, D], F32)
        bfg = sb(gs, "bfg", [8, 1], F32)
        bgt = sb(gs, "bgt", [128, 16], F32)
        gs08 = sb(gs, "gs08", [128, 1], F32)
        brt = sb(gs, "brt", [128, 36], F32)
        lam_t = sb(gs, "lam_t", [1, 256], F32)
        lam_p = sb(gs, "lam_p", [1, 128], F32)
        lam_s = sb(gs, "lam_s", [1, 4], F32)
        neglam = sb(gs, "neglam", [1, 1], F32)
        A1_all = sb(gs, "A1_all", [128, 16, 32], F32)
        A2_all = sb(gs, "A2_all", [128, 16, 32], F32)
        WT = sb(gs, "WT", [128, 16, 2], F32)
        DESTi = sb(gs, "DESTi", [128, 16, 2], I32)
        idx13 = sb(gs, "idx13", [128, NBLK, 4], I32)
        idx2 = sb(gs, "idx2", [128, NBLK, 2], I32)
        junk = sb(gs, "junk", [128, D], BF16)

        S.dma("pool", ident[:], identd, writes=["ident"])
        S.dma("sp", gmix[:], g_mix_b, writes=["gmix"])
        S.dma("sp", gmoe[:], g_moe_b, writes=["gmoe"])
        S.dma("sp", gfin[:], g_fin_b, writes=["gfin"])
        S.dma("sp", bfg[:], b_fg, writes=["bfg"])
        S.dma("sp", bgt[:], b_gate, writes=["bgt"])
        S.dma("sp", gs08[:], g_sub, writes=["gs08"])
        S.dma("sp", brt[:], b_rt, writes=["brt"])
        S.dma("sp", lam_t[:], lamv, writes=["lam_t"])
        S.op("dve", mset(ones_f[:], 1.0), writes=["ones_f"])
        S.op("dve", mset(ones_c[:], 1.0), writes=["ones_c"])
        S.op("dve", ts(gs08[:], gs08[:], 1.0 - LAM_INIT, None, ALU.mult), reads=["gs08"], writes=["gs08"])
        S.op("dve", tt(lam_p[:, 0:64], lam_t[:, 0:64], lam_t[:, 64:128], ALU.mult), reads=["lam_t"], writes=["lam_p"])
        S.op("dve", tt(lam_p[:, 64:128], lam_t[:, 128:192], lam_t[:, 192:256], ALU.mult), reads=["lam_t", "lam_p"],
             writes=["lam_p"])
        S.op("dve", lambda e: e.reduce_sum(out=lam_s[:, 0:1], in_=lam_p[:, 0:64], axis=AX.X), reads=["lam_p"],
             writes=["lam_s"])
        S.op("dve", lambda e: e.reduce_sum(out=lam_s[:, 1:2], in_=lam_p[:, 64:128], axis=AX.X),
             reads=["lam_p", "lam_s"], writes=["lam_s"])
        S.op("act", act(lam_s[:, 2:4], lam_s[:, 0:2], AF.Exp), reads=["lam_s"], writes=["lam_s2"])
        S.op("dve", tt(neglam[:], lam_s[:, 3:4], lam_s[:, 2:3], ALU.subtract), reads=["lam_s2"], writes=["neglam"])
        S.op("dve", ts(neglam[:], neglam[:], -LAM_INIT, None, ALU.add), reads=["neglam"], writes=["neglam"])
        S.flush("setup")

        def norm_tile(xt_ap, xkey, gtile, gkey, hb, hbkey, st, stkey, out_dt_tile=None):
            S.op("act", act(junk[:], xt_ap, AF.Square, accum=st[:, 0:1]), reads=[xkey], writes=[stkey + "a", "junk"])
            S.op("act", act(st[:, 1:2], st[:, 0:1], AF.Sqrt, bias=EPS, scale=1.0 / D), reads=[stkey + "a"],
                 writes=[stkey + "b"])
            S.op("dve", rcp(st[:, 2:3], st[:, 1:2]), reads=[stkey + "b"], writes=[stkey + "c"])
            S.op("dve", stt(hb, xt_ap, st[:, 2:3], gtile[:], ALU.mult, ALU.mult), reads=[xkey, stkey + "c", gkey],
                 writes=[hbkey])

        with contextlib.ExitStack() as st1:
            Fc = sb(st1, "Fc", [8, SEQ], F32)
            ones8 = sb(st1, "ones8", [8, 512], F32)
            S.op("dve", mset(ones8[:], 1.0), writes=["ones8"])
            with contextlib.ExitStack() as p1:
                Wk = sb(p1, "Wk", [128, 8, 1024], BF16)
                Wv = sb(p1, "Wv", [128, 8, 1024], BF16)
                Wf = sb(p1, "Wf", [128, 8, 8], BF16)
                xt = [sb(p1, "xt%d" % i, [128, D], F32) for i in range(4)]
                hb = [sb(p1, "hb%d" % i, [128, D], BF16) for i in range(4)]
                stt_ = [sb(p1, "st%d" % i, [128, 4], F32) for i in range(4)]
                hT = [sb(p1, "hT%d" % i, [128, 8, 512], BF16) for i in range(2)]
                kst = [sb(p1, "kst%d" % i, [128, 512], BF16) for i in range(2)]
                vsf = [sb(p1, "vsf%d" % i, [128, 8, 65], BF16) for i in range(2)]
                vsd = [sb(p1, "vsd%d" % i, [128, 512], BF16) for i in range(2)]
                fs1 = sb(p1, "fs1", [8, 512], F32)
                lf = sb(p1, "lf", [8, 512], F32)
                fr = sb(p1, "fr", [8, 512], F32)
                fsp = [sb(p1, "fsp%d" % i, [8, 3, 512], BF16) for i in range(2)]
                ps_t = [ps(p1, "ps_t%d" % i, [128, 1024], BF16) for i in range(2)]
                ps_k = [ps(p1, "ps_k%d" % i, [128, 512]) for i in range(2)]
                ps_f = ps(p1, "ps_f", [128, 512])
                ps_v = [ps(p1, "ps_v%d" % i, [128, 512]) for i in range(3)]

                wsrc = w_in.rearrange("(c p) n -> p c n", p=128)
                for (dst, lo, wkey) in ((Wk[:, :, 0:512], C_FK, "Wk"), (Wk[:, :, 512:1024], C_DK, "Wk"),
                                        (Wv[:, :, 0:512], C_FV, "Wv"), (Wv[:, :, 512:1024], C_DV, "Wv")):
                    for hh in range(2):
                        S.dma("pool", dst[:, 4 * hh:4 * hh + 4, :], wsrc[:, 4 * hh:4 * hh + 4, lo:lo + 512],
                              writes=[wkey])
                S.dma("pool", Wf[:], wsrc[:, :, C_FF:C_FF + 8], writes=["Wf"])
                for i in range(2):
                    S.op("dve", mset(vsf[i][:], 1.0), writes=["vsf%d" % i])

                vstate = {"vcnt": 0}

                def norm_part(G, r):
                    T = 4 * G + r
                    S.dma("pool", xt[r][:], xs[T * 128:(T + 1) * 128, :], writes=["xt%d" % r])
                    norm_tile(xt[r][:], "xt%d" % r, gmix, "gmix", hb[r][:], "hb%d" % r, stt_[r], "st%d" % r)

                def tr_part(G, r):
                    g2 = G % 2
                    s2 = r % 2
                    for k in range(8):
                        S.op("pe", tr(ps_t[s2][:, k * 128:(k + 1) * 128], hb[r][:, k * 128:(k + 1) * 128], ident[:]),
                             reads=["hb%d" % r, "ident"], writes=["ps_t%d" % s2])
                    S.op("act" if r % 2 == 0 else "dve",
                         (acp if r % 2 == 0 else cp)(hT[g2][:, :, r * 128:(r + 1) * 128],
                                                     ps_t[s2][:, :].rearrange("p (k t) -> p k t", k=8)),
                         reads=["ps_t%d" % s2], writes=["hT%d_%d" % (g2, r)])

                def back(G):
                    g2 = G % 2
                    vcnt = vstate["vcnt"]
                    hkeys = ["hT%d_%d" % (g2, r) for r in range(4)]
                    for cg in range(8):
                        pk = cg % 2
                        for k in range(8):
                            S.op("pe", mm(ps_k[pk][:], Wk[:, k, cg * 128:(cg + 1) * 128], hT[g2][:, k, :], k == 0, k == 7),
                                 reads=hkeys + ["Wk"], writes=["ps_k%d" % pk])
                        S.op("act" if cg % 2 == 0 else "dve", (acp if cg % 2 == 0 else cp)(kst[pk][:], ps_k[pk][:]),
                             reads=["ps_k%d" % pk], writes=["kst%d" % pk])
                        S.dma("sp", KT[2 * cg, :, G * 512:(G + 1) * 512], kst[pk][0:64, :], reads=["kst%d" % pk])
                        S.dma("sp", KT[2 * cg + 1, :, G * 512:(G + 1) * 512], kst[pk][64:128, :], reads=["kst%d" % pk])
                        if cg in (5, 7) and G + 1 < 16:
                            tr_part(G + 1, (cg - 5) // 2)
                    for k in range(8):
                        S.op("pe", mm(ps_f[0:8, :], Wf[:, k, :], hT[g2][:, k, :], k == 0, k == 7),
                             reads=hkeys + ["Wf"], writes=["ps_f"])
                    S.op("act", act(fs1[:], ps_f[0:8, :], AF.Sigmoid, bias=bfg[:, 0:1]), reads=["ps_f", "bfg"],
                         writes=["fs1"])
                    S.op("act", act(lf[:], fs1[:], AF.Ln), reads=["fs1"], writes=["lf"])
                    init = 0.0 if G == 0 else Fc[:, G * 512 - 1:G * 512]
                    S.op("dve", (lambda o, d1, ini: (lambda e: e.tensor_tensor_scan(
                        out=o, data0=ones8[:], data1=d1, initial=ini, op0=ALU.mult, op1=ALU.add)))(
                        Fc[:, G * 512:(G + 1) * 512], lf[:], init),
                        reads=["lf", "ones8", "Fc"], writes=["Fc"])
                    fcur = Fc[:, G * 512:(G + 1) * 512]
                    fk = "fsp%d" % g2
                    S.op("dve", cp(fsp[g2][:, 0, :], fcur), reads=["Fc"], writes=[fk])
                    S.op("dve", tt(fr[:], fcur, fsp[g2][:, 0, :], ALU.subtract), reads=["Fc", fk], writes=["fr"])
                    S.op("dve", cp(fsp[g2][:, 1, :], fr[:]), reads=["fr", fk], writes=[fk])
                    S.op("dve", tt(fr[:], fr[:], fsp[g2][:, 1, :], ALU.subtract), reads=["fr", fk], writes=["fr"])
                    S.op("dve", cp(fsp[g2][:, 2, :], fr[:]), reads=["fr", fk], writes=[fk])
                    S.dma("sp", Fs[:, :, G * 512:(G + 1) * 512], fsp[g2][:], reads=[fk])
                    for r in range(4):
                        T = 4 * G + r
                        s2 = T % 2
                        for half in range(2):
                            pv = vcnt % 3
                            vcnt += 1
                            for k in range(8):
                                S.op("pe", mm(ps_v[pv][:], hT[g2][:, k, r * 128:(r + 1) * 128],
                                              Wv[:, k, half * 512:(half + 1) * 512], k == 0, k == 7),
                                     reads=[hkeys[r], "Wv"], writes=["ps_v%d" % pv])
                            if half == 0:
                                S.op("act", acp(vsf[s2][:, :, 0:64], ps_v[pv][:, :].rearrange("p (h d) -> p h d", h=8)),
                                     reads=["ps_v%d" % pv], writes=["vsf%d" % s2])
                                S.dma("sp", Vf[T], vsf[s2][:, :, :].rearrange("p h d -> p (h d)"), reads=["vsf%d" % s2])
                            else:
                                S.op("dve", cp(vsd[s2][:], ps_v[pv][:]), reads=["ps_v%d" % pv], writes=["vsd%d" % s2])
                                S.dma("sp", Vd[T], vsd[s2][:], reads=["vsd%d" % s2])
                        if r in (0, 1) and G + 1 < 16:
                            tr_part(G + 1, 2 + r)
                    vstate["vcnt"] = vcnt

                for r in range(4):
                    norm_part(0, r)
                for r in range(4):
                    tr_part(0, r)
                for G in range(16):
                    if G + 1 < 16:
                        for r in range(4):
                            norm_part(G + 1, r)
                    back(G)

                S.flush("p1")

            with contextlib.ExitStack() as p2:
                Wq = sb(p2, "Wq", [128, 8, 1024], BF16)
                zt = sb(p2, "zt", [128, 4096], BF16)
                S.op("dve", mset(zt[:], 0.0), writes=["zt"])
                Xz = Xs.rearrange("(p a) d -> p (a d)", p=128)
                zstate = {"i": 0}
                NZ = NROWS * D // 128 // 4096
                xt = [sb(p2, "xt%d" % i, [128, D], F32) for i in range(4)]
                hb = [sb(p2, "hb%d" % i, [128, D], BF16) for i in range(4)]
                stt_ = [sb(p2, "st%d" % i, [128, 4], F32) for i in range(4)]
                hT = [sb(p2, "hT%d" % i, [128, 8, 512], BF16) for i in range(2)]
                qst = [sb(p2, "qst%d" % i, [128, 512], BF16) for i in range(2)]
                fqa = sb(p2, "fqa", [8, 512], F32)
                fr = sb(p2, "fr", [8, 512], F32)
                fsp = [sb(p2, "fsp%d" % i, [8, 3, 512], BF16) for i in range(2)]
                ps_t = [ps(p2, "ps_t%d" % i, [128, 1024], BF16) for i in range(2)]
                ps_k = [ps(p2, "ps_k%d" % i, [128, 512]) for i in range(2)]
                wsrc = w_in.rearrange("(c p) n -> p c n", p=128)
                for (dst, lo) in ((Wq[:, :, 0:512], C_FQ), (Wq[:, :, 512:1024], C_DQ)):
                    for hh in range(2):
                        S.dma("pool", dst[:, 4 * hh:4 * hh + 4, :], wsrc[:, 4 * hh:4 * hh + 4, lo:lo + 512],
                              writes=["W"])
                def norm_part2(m_, r):
                    T = 4 * m_ + r
                    S.dma("pool", xt[r][:], xq[T * 128:(T + 1) * 128, :], writes=["xt%d" % r])
                    norm_tile(xt[r][:], "xt%d" % r, gmix, "gmix", hb[r][:], "hb%d" % r, stt_[r], "st%d" % r)

                def tr_part2(m_, r):
                    g2_ = m_ % 2
                    s2 = r % 2
                    for k in range(8):
                        S.op("pe", tr(ps_t[s2][:, k * 128:(k + 1) * 128], hb[r][:, k * 128:(k + 1) * 128], ident[:]),
                             reads=["hb%d" % r, "ident"], writes=["ps_t%d" % s2])
                    S.op("act" if r % 2 == 0 else "dve",
                         (acp if r % 2 == 0 else cp)(hT[g2_][:, :, r * 128:(r + 1) * 128],
                                                     ps_t[s2][:, :].rearrange("p (k t) -> p k t", k=8)),
                         reads=["ps_t%d" % s2], writes=["hT%d_%d" % (g2_, r)])

                for r in range(4):
                    norm_part2(0, r)
                for r in range(4):
                    tr_part2(0, r)
                for m in range(4):
                    g2 = m % 2
                    if m + 1 < 4:
                        for r in range(4):
                            norm_part2(m + 1, r)
                    hkeys = ["hT%d_%d" % (g2, r) for r in range(4)]
                    for cg in range(8):
                        pk = cg % 2
                        for k in range(8):
                            S.op("pe", mm(ps_k[pk][:], Wq[:, k, cg * 128:(cg + 1) * 128], hT[g2][:, k, :], k == 0, k == 7),
                                 reads=hkeys + ["W"], writes=["ps_k%d" % pk])
                        S.op("act", lambda e, o=qst[pk][:], i=ps_k[pk][:]: e.mul(out=o, in_=i, mul=0.125),
                             reads=["ps_k%d" % pk], writes=["qst%d" % pk])
                        S.dma("sp", QTs[2 * cg, :, m * 512:(m + 1) * 512], qst[pk][0:64, :], reads=["qst%d" % pk])
                        S.dma("sp", QTs[2 * cg + 1, :, m * 512:(m + 1) * 512], qst[pk][64:128, :], reads=["qst%d" % pk])
                        if cg % 2 == 1 and m + 1 < 4:
                            tr_part2(m + 1, (cg - 1) // 2)
                        if zstate["i"] < NZ:
                            zi = zstate["i"]
                            zstate["i"] += 1
                            S.dma("sp", Xz[:, zi * 4096:(zi + 1) * 4096], zt[:], reads=["zt"])
                    S.op("dve", cp(fqa[:], Fc[:, (4 * m + 3) * 512:(4 * m + 4) * 512]), writes=["fqa"])
                    fk = "fsp%d" % g2
                    S.op("dve", cp(fsp[g2][:, 0, :], fqa[:]), reads=["fqa"], writes=[fk])
                    S.op("dve", tt(fr[:], fqa[:], fsp[g2][:, 0, :], ALU.subtract), reads=["fqa", fk], writes=["fr"])
                    S.op("dve", cp(fsp[g2][:, 1, :], fr[:]), reads=["fr", fk], writes=[fk])
                    S.op("dve", tt(fr[:], fr[:], fsp[g2][:, 1, :], ALU.subtract), reads=["fr", fk], writes=["fr"])
                    S.op("dve", cp(fsp[g2][:, 2, :], fr[:]), reads=["fr", fk], writes=[fk])
                    S.dma("sp", Fq[:, :, m * 512:(m + 1) * 512], fsp[g2][:], reads=[fk])
                S.flush("p2")

        with contextlib.ExitStack() as p3:
            KTt = [sb(p3, "KTt%d" % i, [72, SEQ], BF16) for i in range(3)]
            QTt = [sb(p3, "QTt%d" % i, [72, NQ], BF16) for i in range(3)]
            Vall = sb(p3, "Vall", [128, 64, 520], BF16)
            mskf = sb(p3, "mskf", [128, 4, 512], BF16)
            mskd = sb(p3, "mskd", [128, 4, 4, 512], BF16)
            pt = [sb(p3, "pt%d" % i, [128, 512], BF16) for i in range(6)]
            pacc = [sb(p3, "pacc%d" % i, [128, 512], F32) for i in range(2)]
            pacc2 = [sb(p3, "pacc2_%d" % i, [128, 512], F32) for i in range(1)]
            accb = [sb(p3, "accb%d" % i, [128, 512], BF16) for i in range(3)]
            Os = sb(p3, "Os", [128, 512], F32)
            O1 = sb(p3, "O1", [128, 4, 512], F32)
            dn = sb(p3, "dn", [1, 512], F32)
            rdd = [sb(p3, "rdd%d" % i, [1, 512], F32) for i in range(2)]
            t1 = sb(p3, "t1", [128, 512], F32)
            t2 = sb(p3, "t2", [128, 512], F32)
            rd = t2
            rs = sb(p3, "rs", [128, 512], F32)
            Ost = [sb(p3, "Ost%d" % i, [128, 512], BF16) for i in range(1)]
            ps_s = [ps(p3, "ps_s%d" % i, [128, 512]) for i in range(4)]
            ps_o = ps(p3, "ps_o", [128, 512])
            ps_d = ps(p3, "ps_d", [128, 512])
            ps_b = [ps(p3, "ps_b%d" % i, [128, 512]) for i in range(2)]

            state = {"s": 0, "ost": 0, "pa": 0, "p": 0}

            def load_unit(u, slot, kaug_parts, qaug_parts):
                kk = "KTt%d" % slot
                qk = "QTt%d" % slot
                for c in range(4):
                    S.dma("sp", KTt[slot][0:64, c * 2048:(c + 1) * 2048], KT[u, :, c * 2048:(c + 1) * 2048],
                          writes=[kk + "_%d" % c])
                S.dma("sp", QTt[slot][0:64, :], QTs[u], writes=[qk + "_0"])
                for (q, lo, hi, src) in kaug_parts:
                    S.dma(q, KTt[slot][lo:hi, :], src, writes=[kk + "_a%d" % lo])
                for (q, lo, hi, src) in qaug_parts:
                    S.dma(q, QTt[slot][lo:hi, :], src, writes=[qk + "_a%d" % lo])
                kkeys = [kk + "_%d" % c for c in range(4)] + [kk + "_a%d" % lo for (_, lo, _, _) in kaug_parts]
                qkeys = [qk + "_0"] + [qk + "_a%d" % lo for (_, lo, _, _) in qaug_parts]
                return kkeys, qkeys

            def attn_pass(slot, R, kkeys, qkeys, m, mget, vget, M, sep_den, Odst, Okey, ddst, dkey, hook=None):
                n = 16 * m + 16
                slots = {}
                dve_idx = [[i_ for i_ in range(n) if i_ % 6 in (0, 2, 4) and (i_ // 2) % 2 == w_] for w_ in range(2)]
                pool_idx = [i_ for i_ in range(n) if i_ % 6 in (1, 3)]
                pa = state["pa"] % 2
                state["pa"] += 1

                def emit_S(i):
                    s = state["s"] % 4
                    state["s"] += 1
                    slots[i] = s
                    diag = i >= 16 * m + 12
                    S.op("pe", mm(ps_s[s][:], KTt[slot][0:R, i * 128:(i + 1) * 128], QTt[slot][0:R, m * 512:(m + 1) * 512],
                                  True, not diag), reads=kkeys + qkeys, writes=["ps_s%d" % s])
                    if diag:
                        S.op("pe", mm(ps_s[s][:], ident[:], mget(i - 16 * m - 12), False, True),
                             reads=["ident", "msk"], writes=["ps_s%d" % s])

                emit_S(0)
                emit_S(1)
                emit_S(2)
                for i in range(n):
                    s = slots[i]
                    p = state["p"] % 6
                    state["p"] += 1
                    S.op("act", act(pt[p][:], ps_s[s][:], AF.Exp), reads=["ps_s%d" % s], writes=["pt%d" % p])
                    if hook is not None:
                        for (hi_, hf_) in hook:
                            if hi_ == i:
                                hf_()
                    if i + 3 < n:
                        emit_S(i + 3)
                    S.op("pe", mm(ps_o[0:M, :], vget(i), pt[p][:], i == 0, i == n - 1), reads=["pt%d" % p, "Vall"],
                         writes=["ps_o"])
                    if sep_den:
                        if i % 6 == 5:
                            S.op("pe", mm(ps_d[0:1, :], ones_c[:], pt[p][:], i == 5, False),
                                 reads=["pt%d" % p, "ones_c"], writes=["ps_d"])
                        else:
                            if i % 6 in (1, 3):
                                eng_, acc_, ak_, lst_, ab_, abk_ = "pool", pacc2[0], "pacc2_0", pool_idx, accb[2], "accb2"
                            else:
                                w_ = (i // 2) % 2
                                eng_, acc_, ak_, lst_, ab_, abk_ = "dve", pacc[w_], "pacc%d" % w_, dve_idx[w_], accb[w_], "accb%d" % w_
                            if i == lst_[0]:
                                S.op(eng_, cp(acc_[:], pt[p][:]), reads=["pt%d" % p], writes=[ak_])
                            elif i == lst_[-1]:
                                S.op(eng_, tt(ab_[:], acc_[:], pt[p][:], ALU.add),
                                     reads=["pt%d" % p, ak_], writes=[abk_])
                            else:
                                S.op(eng_, tt(acc_[:], acc_[:], pt[p][:], ALU.add),
                                     reads=["pt%d" % p, ak_], writes=[ak_])
                S.op("act", acp(Odst, ps_o[0:M, :]), reads=["ps_o"], writes=[Okey])
                if sep_den:
                    for w_ in range(3):
                        S.op("pe", mm(ps_d[0:1, :], ones_c[:], accb[w_][:], False, w_ == 2),
                             reads=["accb%d" % w_, "ones_c"], writes=["ps_d"])
                    S.op("act", acp(ddst, ps_d[0:1, :]), reads=["ps_d"], writes=[dkey])

            S.dma("pool", mskf[:], mask_f, writes=["msk"])
            for h in range(4):
                S.dma("pool", mskd[:, h, :, :], mask_d[h], writes=["msk"])
            for c in range(4):
                S.dma("sp", Vall[:, 16 * c:16 * c + 16, :], Vf[16 * c:16 * c + 16].rearrange("t p f -> p t f"),
                      writes=["Vall"])

            def unit_loader(uu):
                slot = uu % 3
                if uu < 8:
                    return load_unit(uu, slot,
                                     [("sp", 64, 67, Fs[uu]), ("pool", 67, 71, kaug_f)],
                                     [("pool", 64, 68, neg4), ("sp", 68, 71, Fq[uu])])
                h = (uu - 8) // 2
                return load_unit(uu, slot, [("pool", 64, 69, kaug_d[h])], [("pool", 64, 69, qaug_d[h])])

            loaded = {0: unit_loader(0)}
            pend = {}
            for uu in range(16):
                slot = uu % 3
                if uu + 1 < 16:
                    loaded[uu + 1] = unit_loader(uu + 1)
                kkeys, qkeys = loaded[uu]
                if uu == 8:
                    for c in range(4):
                        S.dma("sp", Vall[:, 16 * c:16 * c + 16, 0:512],
                              Vd[16 * c:16 * c + 16].rearrange("t p f -> p t f"), writes=["Vall"])
                if uu < 8:
                    u = uu
                    for m in range(4):
                        attn_pass(slot, 71, kkeys, qkeys, m, lambda t: mskf[:, t, :],
                                  lambda i, u=u: Vall[:, i, u * 65:(u + 1) * 65], 65, False,
                                  Os[0:65, :], "Os", None, None, hook=pend.pop("f", None))

                        def post_fox_a():
                            S.op("dve", rcp(rd[64:65, :], Os[64:65, :]), reads=["Os"], writes=["t2"])

                        def post_fox_b(u=u, m=m):
                            S.op("pe", mm(ps_b[0][0:64, :], ones_f[64:65, 0:64], rd[64:65, :], True, True),
                                 reads=["t2", "ones_f"], writes=["ps_b0"])
                            o = 0
                            state["ost"] += 1
                            S.op("dve", tt(Ost[o][0:64, :], Os[0:64, :], ps_b[0][0:64, :], ALU.mult),
                                 reads=["Os", "ps_b0"], writes=["Ost%d" % o])
                            S.dma("sp", OAd[u, :, m * 512:(m + 1) * 512], Ost[o][0:64, :], reads=["Ost%d" % o])
                        pend["f"] = [(1, post_fox_a), (9, post_fox_b)]
                    continue
                h = (uu - 8) // 2
                half = (uu - 8) % 2
                for m in range(4):
                    attn_pass(slot, 69, kkeys, qkeys, m, lambda t, h=h: mskd[:, h, t, :],
                              lambda i, h=h: Vall[:, i, h * 128:(h + 1) * 128], 128, True,
                              Os[:], "Os", dn[:], "dn", hook=pend.pop("f", None))

                    def post_diff_a(half=half):
                        S.op("dve", rcp(rdd[half][:], dn[:]), reads=["dn"], writes=["rdd%d" % half])
                        if half == 1:
                            S.op("dve", ts(rdd[1][:], rdd[1][:], neglam[0:1, 0:1], None, ALU.mult),
                                 reads=["rdd1", "neglam"], writes=["rdd1"])

                    def post_diff_b(half=half, m=m):
                        S.op("pe", mm(ps_b[half][:], ones_f[0:1, :], rdd[half][:], True, True),
                             reads=["rdd%d" % half, "ones_f"], writes=["ps_b%d" % half])
                        if half == 0:
                            S.op("dve", tt(O1[:, m, :], Os[:], ps_b[0][:], ALU.mult), reads=["Os", "ps_b0"],
                                 writes=["O1_%d" % m])
                            return
                        S.op("dve", tt(t2[:], Os[:], ps_b[1][:], ALU.mult), reads=["Os", "ps_b1"], writes=["t2"])
                        S.op("dve", tt(t1[:], O1[:, m, :], t2[:], ALU.add), reads=["O1_%d" % m, "t2"], writes=["t1"])
                        S.op("act", act(t2[:], t1[:], AF.Square), reads=["t1"], writes=["t2"])

                    def post_diff_c(h=h, half=half, m=m):
                        if half == 0:
                            return
                        S.op("pe", mm(ps_b[0][:], ones_f[:], t2[:], True, True), reads=["t2", "ones_f"],
                             writes=["ps_b0"])
                        S.op("act", act(rs[:], ps_b[0][:], AF.Sqrt, bias=EPS, scale=1.0 / 128.0), reads=["ps_b0"],
                             writes=["rs"])
                        S.op("dve", rcp(rs[:], rs[:]), reads=["rs"], writes=["rs"])
                        o = 0
                        state["ost"] += 1
                        S.op("dve", stt(Ost[o][:], t1[:], gs08[:, 0:1], rs[:], ALU.mult, ALU.mult),
                             reads=["t1", "gs08", "rs"], writes=["Ost%d" % o])
                        S.dma("sp", OBd[h, :, m * 512:(m + 1) * 512], Ost[o][:], reads=["Ost%d" % o])
                    pend["f"] = [(1, post_diff_a), (8, post_diff_b), (14, post_diff_c)]
            for (_, hf_) in pend.pop("f"):
                hf_()
            S.flush("p3")

        st4 = gs.enter_context(contextlib.ExitStack())
        H2tok = sb(st4, "H2tok", [128, 16, D], BF16)
        with contextlib.ExitStack() as p4:
            Wpa = sb(p4, "Wpa", [64, 8, D], BF16)
            Wpb = sb(p4, "Wpb", [128, 4, D], BF16)
            Wg = sb(p4, "Wg", [128, 8, 2048], BF16)
            Wo = sb(p4, "Wo", [128, 8, D], BF16)
            Wr = sb(p4, "Wr", [128, 8, 36], BF16)
            xt4 = [sb(p4, "xt4_%d" % i, [128, D], F32) for i in range(2)]
            xr4 = [sb(p4, "xr4_%d" % i, [128, D], F32) for i in range(1)]
            hb = [sb(p4, "hb%d" % i, [128, D], BF16) for i in range(4)]
            stt_ = [sb(p4, "st%d" % i, [128, 4], F32) for i in range(3)]
            hT = [sb(p4, "hT%d" % i, [128, 8, 512], BF16) for i in range(2)]
            OAm = [sb(p4, "OAm%d" % i, [64, 8, 512], BF16) for i in range(1)]
            OBm = [sb(p4, "OBm%d" % i, [128, 4, 512], BF16) for i in range(1)]
            ga_s = sb(p4, "ga_s", [128, 512], F32)
            gb_s = sb(p4, "gb_s", [128, 512], F32)
            yT = sb(p4, "yT", [128, 8, 512], BF16)
            xm = [sb(p4, "xm%d" % i, [128, D], F32) for i in range(2)]
            h2T = [sb(p4, "h2T%d" % i, [128, 8, 512], BF16) for i in range(1)]
            lg = sb(p4, "lg", [128, 36], F32)
            rt = sb(p4, "rt", [128, 16], F32)
            pen = sb(p4, "pen", [128, 4], F32)
            elm = sb(p4, "elm", [128, 32], F32)
            m8 = sb(p4, "m8", [128, 8], F32)
            ge = sb(p4, "ge", [128, 4], F32)
            ps_t = ps(p4, "ps_t", [128, 1024], BF16)
            ps_a = ps(p4, "ps_a", [128, 512])
            ps_bb = ps(p4, "ps_bb", [128, 512])
            ps_za = ps(p4, "ps_za", [128, 512])
            ps_zb = ps(p4, "ps_zb", [128, 512])
            ps_o0 = ps(p4, "ps_o0", [128, 512])
            ps_o1 = ps(p4, "ps_o1", [128, 512])
            ps_r = ps(p4, "ps_r", [128, 512])

            for hh in range(2):
                S.dma("pool", Wpa[:, 4 * hh:4 * hh + 4, :],
                      w_pa.rearrange("(h p) n -> p h n", p=64)[:, 4 * hh:4 * hh + 4, :], writes=["W"])
            S.dma("pool", Wpb[:], w_pb.rearrange("(h p) n -> p h n", p=128), writes=["W"])
            wsrc = w_in.rearrange("(c p) n -> p c n", p=128)
            for cc in range(4):
                for hh in range(2):
                    S.dma("pool", Wg[:, 4 * hh:4 * hh + 4, cc * 512:(cc + 1) * 512],
                          wsrc[:, 4 * hh:4 * hh + 4, C_GA + cc * 512:C_GA + (cc + 1) * 512], writes=["W"])
            wo_src = w_o.rearrange("(c p) n -> p c n", p=128)
            for hh in range(2):
                S.dma("pool", Wo[:, 4 * hh:4 * hh + 4, :], wo_src[:, 4 * hh:4 * hh + 4, :], writes=["W"])
            S.dma("pool", Wr[:], w_rt.rearrange("(c p) n -> p c n", p=128), writes=["W"])

            def norm_part4(m_, r):
                T = 4 * m_ + r
                s3 = T % 3
                s2 = r % 2
                S.dma("pool", xt4[s2][:], xq[T * 128:(T + 1) * 128, :], writes=["xt4_%d" % s2])
                norm_tile(xt4[s2][:], "xt4_%d" % s2, gmix, "gmix", hb[r][:], "hb%d" % r, stt_[s3], "st%d" % s3)

            def tr_part4(m_, r):
                hs = m_ % 2
                for k in range(8):
                    S.op("pe", tr(ps_t[:, k * 128:(k + 1) * 128], hb[r][:, k * 128:(k + 1) * 128], ident[:]),
                         reads=["hb%d" % r, "ident"], writes=["ps_t"])
                S.op("act", acp(hT[hs][:, :, r * 128:(r + 1) * 128], ps_t[:, :].rearrange("p (k t) -> p k t", k=8)),
                     reads=["ps_t"], writes=["hT%d_%d" % (hs, r)])

            for r in range(4):
                norm_part4(0, r)
                tr_part4(0, r)
            for m in range(4):
                g2 = 0
                hs = m % 2
                S.dma("pool", OAm[g2][:], OAd[:, :, m * 512:(m + 1) * 512].rearrange("h p t -> p h t"),
                      writes=["OAm%d" % g2])
                S.dma("pool", OBm[g2][:], OBd[:, :, m * 512:(m + 1) * 512].rearrange("h p t -> p h t"),
                      writes=["OBm%d" % g2])
                hkeys = ["hT%d_%d" % (hs, r) for r in range(4)]
                for c in range(8):
                    if m + 1 < 4:
                        if c < 4:
                            norm_part4(m + 1, c)
                        else:
                            tr_part4(m + 1, c - 4)
                    if c % 2 == 0:
                        A_, Ak, B_, Bk, ZA, ZAk = ps_a, "ps_a", ps_bb, "ps_bb", ps_za, "ps_za"
                    else:
                        A_, Ak, B_, Bk, ZA, ZAk = ps_o0, "ps_o0", ps_o1, "ps_o1", ps_r, "ps_r"
                    for hh in range(8):
                        S.op("pe", mm(A_[:], Wpa[:, hh, c * 128:(c + 1) * 128], OAm[g2][:, hh, :], hh == 0, hh == 7),
                             reads=["W", "OAm%d" % g2], writes=[Ak])
                    for hh in range(4):
                        S.op("pe", mm(B_[:], Wpb[:, hh, c * 128:(c + 1) * 128], OBm[g2][:, hh, :], hh == 0, hh == 3),
                             reads=["W", "OBm%d" % g2], writes=[Bk])
                    for k in range(8):
                        S.op("pe", mm(ZA[:], Wg[:, k, c * 128:(c + 1) * 128], hT[hs][:, k, :], k == 0, k == 7),
                             reads=hkeys + ["W"], writes=[ZAk])
                    for k in range(8):
                        S.op("pe", mm(ps_zb[:], Wg[:, k, 1024 + c * 128:1024 + (c + 1) * 128], hT[hs][:, k, :], k == 0,
                                      k == 7), reads=hkeys + ["W"], writes=["ps_zb"])
                    S.op("act", act(ga_s[:], ZA[:], AF.Sigmoid, bias=bgt[:, c:c + 1]), reads=[ZAk, "bgt"],
                         writes=["ga_s"])
                    S.op("act", act(gb_s[:], ps_zb[:], AF.Sigmoid, bias=bgt[:, 8 + c:9 + c]), reads=["ps_zb", "bgt"],
                         writes=["gb_s"])
                    S.op("dve", tt(ga_s[:], ga_s[:], A_[:], ALU.mult), reads=["ga_s", Ak], writes=["ga_s"])
                    S.op("dve", tt(gb_s[:], gb_s[:], B_[:], ALU.mult), reads=["gb_s", Bk], writes=["gb_s"])
                    S.op("dve", tt(yT[:, c, :], ga_s[:], gb_s[:], ALU.add), reads=["ga_s", "gb_s"], writes=["yT_%d" % c])
                ykeys = ["yT_%d" % c for c in range(8)]

                def o_mm(r):
                    T_ = 4 * m + r
                    S.dma("pool", xr4[0][:], xq[T_ * 128:(T_ + 1) * 128, :], writes=["xr4_0"])
                    for half, pso, pk in ((0, ps_o0, "ps_o0"), (1, ps_o1, "ps_o1")):
                        for c in range(8):
                            S.op("pe", mm(pso[:], yT[:, c, r * 128:(r + 1) * 128], Wo[:, c, half * 512:(half + 1) * 512],
                                          c == 0, c == 7), reads=ykeys + ["W"], writes=[pk])

                def o_add(r):
                    T = 4 * m + r
                    s2 = T % 2
                    xk = "xr4_0"
                    for half, pso, pk in ((0, ps_o0, "ps_o0"), (1, ps_o1, "ps_o1")):
                        S.op("dve", tt(xm[s2][:, half * 512:(half + 1) * 512], pso[:],
                                       xr4[0][:, half * 512:(half + 1) * 512], ALU.add), reads=[pk, xk],
                             writes=["xm%d_%d" % (s2, half)])

                def o_rest(r):
                    T = 4 * m + r
                    s3 = T % 3
                    s2 = T % 2
                    xmk = ["xm%d_0" % s2, "xm%d_1" % s2]
                    S.dma("sp", XM[T * 128:(T + 1) * 128, :], xm[s2][:], reads=xmk)
                    S.op("act", act(junk[:], xm[s2][:], AF.Square, accum=stt_[s3][:, 0:1]), reads=xmk,
                         writes=["st%da" % s3, "junk"])
                    S.op("act", act(stt_[s3][:, 1:2], stt_[s3][:, 0:1], AF.Sqrt, bias=EPS, scale=1.0 / D),
                         reads=["st%da" % s3], writes=["st%db" % s3])
                    S.op("dve", rcp(stt_[s3][:, 2:3], stt_[s3][:, 1:2]), reads=["st%db" % s3], writes=["st%dc" % s3])
                    S.op("dve", stt(H2tok[:, T, :], xm[s2][:], stt_[s3][:, 2:3], gmoe[:], ALU.mult, ALU.mult),
                         reads=xmk + ["st%dc" % s3, "gmoe"], writes=["H2tok%d" % T])
                    for k in range(8):
                        S.op("pe", tr(ps_t[:, k * 128:(k + 1) * 128], H2tok[:, T, k * 128:(k + 1) * 128], ident[:]),
                             reads=["H2tok%d" % T, "ident"], writes=["ps_t"])
                    S.op("act", acp(h2T[g2][:, :, r * 128:(r + 1) * 128], ps_t[:, :].rearrange("p (k t) -> p k t", k=8)),
                         reads=["ps_t"], writes=["h2T%d_%d" % (g2, r)])
                    for k in range(8):
                        S.op("pe", mm(ps_r[:, 0:36], h2T[g2][:, k, r * 128:(r + 1) * 128], Wr[:, k, :], k == 0, k == 7),
                             reads=["h2T%d_%d" % (g2, r), "W"], writes=["ps_r"])
                    S.op("dve", tt(lg[:], ps_r[:, 0:36], brt[:], ALU.add), reads=["ps_r", "brt"], writes=["lg"])
                    S.op("dve", lambda e: e.reduce_max(out=rt[:, 0:1], in_=lg[:, 0:4], axis=AX.X), reads=["lg"],
                         writes=["rt0"])
                    S.op("dve", ts(rt[:, 1:2], rt[:, 0:1], -1.0, None, ALU.mult), reads=["rt0"], writes=["rt1"])
                    S.op("act", act(ge[:], lg[:, 0:4], AF.Exp, bias=rt[:, 1:2], accum=rt[:, 2:3]), reads=["lg", "rt1"],
                         writes=["ge", "rt2"])
                    S.op("dve", rcp(rt[:, 3:4], rt[:, 2:3]), reads=["rt2"], writes=["rt3"])
                    S.op("dve", ts(pen[:], lg[:, 0:4], rt[:, 0:1], 1.0e9, ALU.is_ge, ALU.mult), reads=["lg", "rt0"],
                         writes=["pen"])
                    S.op("dve", ts(pen[:], pen[:], -1.0e9, None, ALU.add), reads=["pen"], writes=["pen"])
                    for g in range(4):
                        S.op("dve", ts(elm[:, 8 * g:8 * g + 8], lg[:, 4 + 8 * g:12 + 8 * g], pen[:, g:g + 1], None, ALU.add),
                             reads=["lg", "pen", "elm"], writes=["elm"])
                    S.op("dve", lambda e: e.max(out=m8[:], in_=elm[:]), reads=["elm"], writes=["m8"])
                    S.op("dve", tt(rt[:, 4:5], m8[:, 0:1], m8[:, 1:2], ALU.subtract), reads=["m8"], writes=["rt4"])
                    S.op("act", act(rt[:, 5:6], rt[:, 4:5], AF.Sigmoid), reads=["rt4"], writes=["rt5"])
                    S.op("dve", ts(rt[:, 6:7], rt[:, 5:6], -1.0, 1.0, ALU.mult, ALU.add), reads=["rt5"], writes=["rt6"])
                    S.op("dve", ts(A1_all[:, T, :], elm[:], m8[:, 0:1], None, ALU.is_equal), reads=["elm", "m8"],
                         writes=["A1_%d" % T])
                    S.op("dve", ts(A2_all[:, T, :], elm[:], m8[:, 1:2], None, ALU.is_equal), reads=["elm", "m8"],
                         writes=["A2_%d" % T])
                    S.op("dve", tt(WT[:, T, 0:1], rt[:, 5:6], rt[:, 3:4], ALU.mult), reads=["rt5", "rt3"],
                         writes=["WT_%d" % T])
                    S.op("dve", tt(WT[:, T, 1:2], rt[:, 6:7], rt[:, 3:4], ALU.mult), reads=["rt6", "rt3", "WT_%d" % T],
                         writes=["WT_%d" % T])

                o_mm(0)
                for r in range(4):
                    o_add(r)
                    if r + 1 < 4:
                        o_mm(r + 1)
                    o_rest(r)
            S.flush("p4")

        IOA = bass.IndirectOffsetOnAxis
        with contextlib.ExitStack() as p4b:
            Ab = sb(p4b, "Ab", [128, 16, 32], BF16)
            Ltri = sb(p4b, "Ltri", [128, 128], BF16)
            onesb = sb(p4b, "onesb", [128, 128], BF16)
            ones32 = sb(p4b, "ones32", [128, 32], F32)
            cnt_s = sb(p4b, "cnt_s", [128, 32], F32)
            nbt = sb(p4b, "nbt", [128, 32], F32)
            padded = sb(p4b, "padded", [128, 32], F32)
            pend = sb(p4b, "pend", [128, 32], F32)
            pstart = sb(p4b, "pstart", [128, 32], F32)
            dst = sb(p4b, "dst", [128, 32], F32)
            tmp = sb(p4b, "tmp", [128, 32], F32)
            DESTf = sb(p4b, "DESTf", [128, 16, 2], F32)
            EBf = sb(p4b, "EBf", [128, NBLK], F32)
            EB1024 = sb(p4b, "EB1024", [128, NBLK], F32)
            EB512 = sb(p4b, "EB512", [128, NBLK], F32)
            cmpb = sb(p4b, "cmpb", [128, 32], F32)
            idx13f = sb(p4b, "idx13f", [128, NBLK, 4], F32)
            idx2f = sb(p4b, "idx2f", [128, NBLK, 2], F32)
            iot13t = sb(p4b, "iot13t", [128, 4], F32)
            iot2t = sb(p4b, "iot2t", [128, 2], F32)
            ps_pos = ps(p4b, "ps_pos", [128, 512])
            ps_cnt = ps(p4b, "ps_cnt", [128, 512])
            S.dma("pool", Ltri[:], ltri, writes=["Ltri"])
            S.dma("sp", iot13t[:], iot13, writes=["iot13t"])
            S.dma("sp", iot2t[:], iot2, writes=["iot2t"])
            S.op("dve", mset(onesb[:], 1.0), writes=["onesb"])
            S.op("dve", mset(ones32[:], 1.0), writes=["ones32"])
            for T in range(16):
                S.op("dve", tt(Ab[:, T, :], A1_all[:, T, :], A2_all[:, T, :], ALU.add), writes=["Ab%d" % T])
            for T in range(16):
                S.op("pe", mm(ps_cnt[:, 0:32], onesb[:], Ab[:, T, :], T == 0, T == 15), reads=["onesb", "Ab%d" % T],
                     writes=["ps_cnt"])
            S.op("dve", cp(cnt_s[:], ps_cnt[:, 0:32]), reads=["ps_cnt"], writes=["cnt_s"])
            S.op("dve", mset(nbt[:], 0.0), writes=["nbt"])
            for k in range(16):
                S.op("dve", stt(nbt[:], cnt_s[:], float(256 * k), nbt[:], ALU.is_gt, ALU.add), reads=["cnt_s", "nbt"],
                     writes=["nbt"])
            S.op("dve", ts(padded[:], nbt[:], 256.0, None, ALU.mult), reads=["nbt"], writes=["padded"])
            S.op("dve", lambda e: e.tensor_tensor_scan(out=pend[:], data0=ones32[:], data1=padded[:], initial=0.0,
                                                       op0=ALU.mult, op1=ALU.add), reads=["ones32", "padded"],
                 writes=["pend"])
            S.op("dve", tt(pstart[:], pend[:], padded[:], ALU.subtract), reads=["pend", "padded"], writes=["pstart"])
            for T in range(16):
                S.op("pe", mm(ps_pos[:, 0:32], Ltri[:], Ab[:, T, :], True, T == 0), reads=["Ltri", "Ab%d" % T],
                     writes=["ps_pos"])
                for T2 in range(T):
                    S.op("pe", mm(ps_pos[:, 0:32], onesb[:], Ab[:, T2, :], False, T2 == T - 1),
                         reads=["onesb", "Ab%d" % T2], writes=["ps_pos"])
                S.op("dve", tt(dst[:], ps_pos[:, 0:32], pstart[:], ALU.add), reads=["ps_pos", "pstart"], writes=["dst"])
                S.op("dve", tt(tmp[:], A1_all[:, T, :], dst[:], ALU.mult), reads=["dst"], writes=["tmp"])
                S.op("dve", lambda e, T=T: e.reduce_sum(out=DESTf[:, T, 0:1], in_=tmp[:], axis=AX.X), reads=["tmp"],
                     writes=["DESTf"])
                S.op("dve", tt(tmp[:], A2_all[:, T, :], dst[:], ALU.mult), reads=["dst", "tmp"], writes=["tmp"])
                S.op("dve", lambda e, T=T: e.reduce_sum(out=DESTf[:, T, 1:2], in_=tmp[:], axis=AX.X),
                     reads=["tmp", "DESTf"], writes=["DESTf"])
            S.op("dve", cp(DESTi[:], DESTf[:]), reads=["DESTf"], writes=["DESTi"])
            for T in range(16):
                for a_ in range(2):
                    S.idma(Xs, IOA(ap=DESTi[:, T, a_:a_ + 1], axis=0), H2tok[:, T, :], None, reads=["DESTi"])
            for b_ in range(NBLK):
                S.op("dve", ts(cmpb[:], pend[:], float(256 * b_), None, ALU.is_le), reads=["pend", "cmpb"],
                     writes=["cmpb"])
                S.op("dve", lambda e, b_=b_: e.reduce_sum(out=EBf[:, b_:b_ + 1], in_=cmpb[:], axis=AX.X),
                     reads=["cmpb", "EBf"], writes=["EBf"])
            S.op("dve", ts(EB1024[:], EBf[:], 512.0, None, ALU.mult), reads=["EBf"], writes=["EB1024"])
            S.op("dve", ts(EB512[:], EBf[:], 256.0, None, ALU.mult), reads=["EBf"], writes=["EB512"])
            for b_ in range(NBLK):
                S.op("dve", ts(idx13f[:, b_, :], iot13t[:], EB1024[:, b_:b_ + 1], None, ALU.add),
                     reads=["iot13t", "EB1024", "idx13f"], writes=["idx13f"])
                S.op("dve", ts(idx2f[:, b_, :], iot2t[:], EB512[:, b_:b_ + 1], None, ALU.add),
                     reads=["iot2t", "EB512", "idx2f"], writes=["idx2f"])
            S.op("dve", cp(idx13[:], idx13f[:]), reads=["idx13f"], writes=["idx13"])
            S.op("dve", cp(idx2[:], idx2f[:]), reads=["idx2f"], writes=["idx2"])
            S.flush("p4b")
        st4.close()

        with contextlib.ExitStack() as p5:
            W13 = [sb(p5, "W13_%d" % i, [128, 8, 1024], BF16) for i in range(2)]
            W2s = [sb(p5, "W2s_%d" % i, [128, 4, D], BF16) for i in range(2)]
            Xb = [sb(p5, "Xb%d" % i, [128, 2, D], BF16) for i in range(2)]
            xT = [sb(p5, "xT%d" % i, [128, 8, 256], BF16) for i in range(2)]
            sl = [sb(p5, "sl%d" % i, [128, 512], F32) for i in range(2)]
            GT = [sb(p5, "GT%d" % i, [128, 1024], BF16) for i in range(2)]
            Ys = [sb(p5, "Ys%d" % i, [128, 2, D], BF16) for i in range(2)]
            ps_t = [ps(p5, "ps_t%d" % i, [128, 1024], BF16) for i in range(2)]
            ps_h1 = [ps(p5, "ps_h1%d" % i, [128, 512]) for i in range(2)]
            ps_h3 = [ps(p5, "ps_h3%d" % i, [128, 512]) for i in range(2)]
            ps_y = [ps(p5, "ps_y%d" % i, [128, 512]) for i in range(2)]

            def issue(b_):
                sl_ = b_ % 2
                for c in range(4):
                    S.idma(W13[sl_][:, 2 * c:2 * c + 2, :].rearrange("p a n -> p (a n)"), None, w13r,
                           IOA(ap=idx13[:, b_, c:c + 1], axis=0),
                           writes=["W13_%d_%d" % (sl_, c)], bound=32 * 128 * 4 - 1)
                for c in range(2):
                    S.idma(W2s[sl_][:, 2 * c:2 * c + 2, :].rearrange("p a n -> p (a n)"), None, w2r,
                           IOA(ap=idx2[:, b_, c:c + 1], axis=0),
                           writes=["W2s_%d_%d" % (sl_, c)], bound=32 * 128 * 2 - 1)
                S.dma("sp", Xb[sl_][:], Xs[b_ * 256:(b_ + 1) * 256, :].rearrange("(t p) d -> p t d", p=128),
                      writes=["Xb%d" % sl_])

            issue(0)
            for b_ in range(NBLK):
                sl_ = b_ % 2
                if b_ + 1 < NBLK:
                    issue(b_ + 1)
                wk13 = ["W13_%d_%d" % (sl_, c) for c in range(4)]
                wk2 = ["W2s_%d_%d" % (sl_, c) for c in range(2)]
                for half in range(2):
                    for k in range(8):
                        S.op("pe", tr(ps_t[half][:, k * 128:(k + 1) * 128], Xb[sl_][:, half, k * 128:(k + 1) * 128],
                                      ident[:]), reads=["Xb%d" % sl_, "ident"], writes=["ps_t%d" % half])
                    S.op("act" if half == 0 else "dve",
                         (acp if half == 0 else cp)(xT[sl_][:, :, half * 128:(half + 1) * 128],
                                                    ps_t[half][:, :].rearrange("p (k t) -> p k t", k=8)),
                         reads=["ps_t%d" % half], writes=["xT%d_%d" % (sl_, half)])
                xk = ["xT%d_0" % sl_, "xT%d_1" % sl_]
                for bank in range(2):
                    for f2 in range(2):
                        fc = 2 * bank + f2
                        for k in range(8):
                            S.op("pe", mm(ps_h1[bank][:, f2 * 256:(f2 + 1) * 256], W13[sl_][:, k, fc * 128:(fc + 1) * 128],
                                          xT[sl_][:, k, :], k == 0, k == 7), reads=wk13 + xk, writes=["ps_h1%d" % bank])
                        for k in range(8):
                            S.op("pe", mm(ps_h3[bank][:, f2 * 256:(f2 + 1) * 256],
                                          W13[sl_][:, k, 512 + fc * 128:512 + (fc + 1) * 128],
                                          xT[sl_][:, k, :], k == 0, k == 7), reads=wk13 + xk, writes=["ps_h3%d" % bank])
                    S.op("act", act(sl[bank][:], ps_h1[bank][:], AF.Silu), reads=["ps_h1%d" % bank],
                         writes=["sl%d" % bank])
                    S.op("dve", tt(GT[sl_][:, bank * 512:(bank + 1) * 512], sl[bank][:], ps_h3[bank][:], ALU.mult),
                         reads=["sl%d" % bank, "ps_h3%d" % bank], writes=["GT%d_%d" % (sl_, bank)])
                gk = ["GT%d_0" % sl_, "GT%d_1" % sl_]
                for half in range(2):
                    for nh in range(2):
                        for fc in range(4):
                            S.op("pe", mm(ps_y[nh][:], GT[sl_][:, fc * 256 + half * 128:fc * 256 + (half + 1) * 128],
                                          W2s[sl_][:, fc, nh * 512:(nh + 1) * 512], fc == 0, fc == 3),
                                 reads=gk + wk2, writes=["ps_y%d" % nh])
                        S.op("act" if nh == 0 else "dve",
                             (acp if nh == 0 else cp)(Ys[sl_][:, half, nh * 512:(nh + 1) * 512], ps_y[nh][:]),
                             reads=["ps_y%d" % nh], writes=["Ys%d_%d_%d" % (sl_, half, nh)])
                S.dma("sp", Yd[b_ * 256:(b_ + 1) * 256, :].rearrange("(t p) d -> p t d", p=128), Ys[sl_][:],
                      reads=["Ys%d_%d_%d" % (sl_, h_, n_) for h_ in range(2) for n_ in range(2)])
            S.flush("p5")

        with contextlib.ExitStack() as p6:
            Y1 = [sb(p6, "Y1_%d" % i, [128, D], BF16) for i in range(2)]
            Y2 = [sb(p6, "Y2_%d" % i, [128, D], BF16) for i in range(2)]
            xmt = [sb(p6, "xmt%d" % i, [128, D], F32) for i in range(2)]
            stt_ = [sb(p6, "st%d" % i, [128, 4], F32) for i in range(2)]
            ot = [sb(p6, "ot%d" % i, [128, D], F32) for i in range(2)]
            def pre6(T):
                s2 = T % 2
                S.idma(Y1[s2][:], None, Yd, IOA(ap=DESTi[:, T, 0:1], axis=0), writes=["Y1_%d" % s2])
                S.idma(Y2[s2][:], None, Yd, IOA(ap=DESTi[:, T, 1:2], axis=0), writes=["Y2_%d" % s2])
                S.dma("sp", xmt[s2][:], XM[T * 128:(T + 1) * 128, :], writes=["xmt%d" % s2])

            pre6(0)
            for T in range(16):
                s2 = T % 2
                if T + 1 < 16:
                    pre6(T + 1)
                S.op("dve", stt(xmt[s2][:], Y1[s2][:], WT[:, T, 0:1], xmt[s2][:], ALU.mult, ALU.add),
                     reads=["Y1_%d" % s2, "xmt%d" % s2], writes=["xmt%d" % s2])
                S.op("dve", stt(xmt[s2][:], Y2[s2][:], WT[:, T, 1:2], xmt[s2][:], ALU.mult, ALU.add),
                     reads=["Y2_%d" % s2, "xmt%d" % s2], writes=["xmt%d" % s2])
                S.op("act", act(junk[:], xmt[s2][:], AF.Square, accum=stt_[s2][:, 0:1]), reads=["xmt%d" % s2],
                     writes=["st%da" % s2, "junk"])
                S.op("act", act(stt_[s2][:, 1:2], stt_[s2][:, 0:1], AF.Sqrt, bias=EPS, scale=1.0 / D),
                     reads=["st%da" % s2], writes=["st%db" % s2])
                S.op("dve", rcp(stt_[s2][:, 2:3], stt_[s2][:, 1:2]), reads=["st%db" % s2], writes=["st%dc" % s2])
                S.op("dve", stt(ot[s2][:], xmt[s2][:], stt_[s2][:, 2:3], gfin[:], ALU.mult, ALU.mult),
                     reads=["xmt%d" % s2, "st%dc" % s2, "gfin"], writes=["ot%d" % s2])
                S.dma("sp", out[T * 128:(T + 1) * 128, :], ot[s2][:], reads=["ot%d" % s2])
            S.flush("p6")
    return nc


def _bf16_round(a):
    a = np.ascontiguousarray(a, dtype=np.float32)
    u = a.view(np.uint32).astype(np.uint64)
    u = (u + 0x7FFF + ((u >> 16) & 1)) & 0xFFFF0000
    return u.astype(np.uint32).view(np.float32)


def _consts():
    slopes = [2.0 ** (-2.0 * (h + 1)) for h in range(4)]
    kpos = np.arange(SEQ, dtype=np.float64)
    qpos = np.concatenate([np.arange((4 * m + 3) * 512, (4 * m + 4) * 512) for m in range(4)]).astype(np.float64)
    kaug = np.zeros((4, 5, SEQ), np.float32)
    qaug = np.zeros((4, 5, NQ), np.float32)
    for h, s in enumerate(slopes):
        v = (s * kpos).astype(np.float32)
        hi = _bf16_round(v)
        kaug[h, 0] = 1.0
        kaug[h, 1] = 1.0
        kaug[h, 2] = hi
        kaug[h, 3] = v - hi
        vq = (-s * qpos).astype(np.float32)
        hq = _bf16_round(vq)
        qaug[h, 0] = hq
        qaug[h, 1] = vq - hq
        qaug[h, 2] = 1.0
        qaug[h, 3] = 1.0
        qaug[h, 4] = -1.0
    ki = np.arange(128)[:, None, None]
    t = np.arange(4)[None, :, None]
    qi = np.arange(512)[None, None, :]
    krel = 128 * t + ki
    qrel = qi + 0 * krel
    mask_f = np.where(krel <= qrel, 0.0, NEG).astype(np.float32)
    mask_d = np.zeros((4, 128, 4, 512), np.float32)
    allowed = (krel // 64) <= (qrel // 64)
    fut = np.maximum(krel - qrel, 0).astype(np.float64)
    for h, s in enumerate(slopes):
        mask_d[h] = np.where(allowed, -2.0 * s * fut, NEG).astype(np.float32)
    return kaug, qaug, mask_f, mask_d


def _in_maps(inp):
    f = np.float32
    x = np.asarray(inp["x"], f)
    w_in = np.ascontiguousarray(np.asarray(inp["w_in"], f)[0])
    tile128 = lambda v: np.ascontiguousarray(np.broadcast_to(np.asarray(v, f).reshape(1, -1), (128, np.asarray(v).size)))
    shared = {
        "w_in": w_in,
        "g_mix_b": tile128(inp["g_mix"][0]),
        "g_moe_b": tile128(inp["g_moe"][0]),
        "g_fin_b": tile128(inp["g_final"]),
        "b_fg": np.ascontiguousarray(np.asarray(inp["b_fgate"], f)[0].reshape(8, 1)),
        "b_gate": np.ascontiguousarray(np.asarray(inp["b_gate"], f)[0].reshape(16, 128).T),
        "lamv": np.ascontiguousarray(np.concatenate([np.asarray(inp[k], f)[0] for k in
                                                     ("lam_q1", "lam_k1", "lam_q2", "lam_k2")]).reshape(1, 256)),
        "g_sub": np.ascontiguousarray(np.asarray(inp["g_subln"], f)[0].reshape(128, 1)),
        "w_pa": np.ascontiguousarray(np.asarray(inp["w_pa"], f)[0]),
        "w_pb": np.ascontiguousarray(np.asarray(inp["w_pb"], f)[0]),
        "w_o": np.ascontiguousarray(np.asarray(inp["w_o"], f)[0]),
        "w_rt": np.ascontiguousarray(np.concatenate([np.asarray(inp["w_group"], f)[0],
                                                     np.asarray(inp["w_expert"], f)[0]], axis=1)),
        "b_rt": tile128(np.concatenate([np.asarray(inp["b_group"], f)[0].reshape(-1),
                                        np.asarray(inp["b_expert"], f)[0].reshape(-1)])),
        "w13r": np.ascontiguousarray(np.concatenate([np.asarray(inp["w1"], f)[0], np.asarray(inp["w3"], f)[0]],
                                                    axis=-1).reshape(32, 8, 128, 1024).transpose(0, 2, 1, 3)
                                     ).reshape(32 * 128 * 4, 2048),
        "w2r": np.ascontiguousarray(np.asarray(inp["w2"], f)[0].reshape(32, 4, 128, 1024).transpose(0, 2, 1, 3)
                                    ).reshape(32 * 128 * 2, 2048),
        "ltri": np.triu(np.ones((128, 128), f), 1),
        "iot13": (np.arange(128, dtype=f)[:, None] * 4 + np.arange(4, dtype=f)[None, :]),
        "iot2": (np.arange(128, dtype=f)[:, None] * 2 + np.arange(2, dtype=f)[None, :]),
        "identd": np.eye(128, dtype=f),
        "neg4": -np.ones((4, NQ), f),
    }
    kaug, qaug, mask_f, mask_d = _consts()
    shared["qaug_d"] = qaug
    shared["mask_f"] = mask_f
    shared["mask_d"] = mask_d
    maps = []
    for core in range(8):
        b, j = core // 4, core % 4
        shift = (3 - j) * 512
        d = dict(shared)
        xsc = np.zeros((SEQ, D), f)
        xsc[shift:] = x[b, :SEQ - shift]
        d["xs"] = xsc
        d["xq"] = np.ascontiguousarray(np.concatenate([x[b, (4 * m + j) * 512:(4 * m + j + 1) * 512] for m in range(4)], 0))
        pad = np.zeros((SEQ,), f)
        pad[:shift] = 1.0e30
        kd = kaug.copy()
        kd[:, 4, :] = pad
        d["kaug_d"] = kd
        kf = np.ones((4, SEQ), f)
        kf[0] = pad
        d["kaug_f"] = kf
        maps.append(d)
    return maps


_NC = {}


def kernel(**inputs):
    if "nc" not in _NC:
        _NC["nc"] = build_nc()
    nc = _NC["nc"]
    maps = _in_maps(inputs)
    res = run_bass_kernel_spmd(nc, maps, core_ids=list(range(8)))
    outp = np.zeros((2, SEQ, D), np.float32)
    for core in range(8):
        b, j = core // 4, core % 4
        o = np.asarray(res.results[core]["out"], np.float32)
        for m in range(4):
            outp[b, (4 * m + j) * 512:(4 * m + j + 1) * 512] = o[m * 512:(m + 1) * 512]
    return outp
```
